# Optimizing a Trainium2 kernel written in Bass

```python
import jax, jax.numpy as jnp
from jax import lax
import numpy as np

D_MODEL = 4096
BATCH = 2
SEQ = 8192
DEPTH = 4

RET_HEADS = 8
RET_DK = 128
RET_DV = 256
RET_QK = RET_HEADS * RET_DK
RET_V = RET_HEADS * RET_DV
RET_CHUNK = 128
ROPE_BASE = 10000.0
LRU_WIDTH = D_MODEL // 2
LRU_BLOCKS = 16
LRU_BW = LRU_WIDTH // LRU_BLOCKS
CONV_WIDTH = 4
LRU_C = 8.0
MIX_WIDTH = RET_V + LRU_WIDTH
IN_WIDTH = 2 * RET_QK + 2 * RET_V + 2 * LRU_WIDTH
SPLITS = (RET_QK, 2 * RET_QK, 2 * RET_QK + RET_V, 2 * RET_QK + 2 * RET_V, 2 * RET_QK + 2 * RET_V + LRU_WIDTH)
D_FF = 2 * D_MODEL
N_EXPERTS = 8
TOP_K = 2
D_EXPERT = 7 * D_MODEL // 16
N_DENSE = (DEPTH + 1) // 2
N_MOE = DEPTH // 2
N_MOD = 6
EPS = 1e-6

kernel_name = "hybrid_retention_rglru_moe_adaln"


def rms_norm(x):
    xf = x.astype(jnp.float32)
    return (xf * lax.rsqrt(jnp.mean(xf * xf, axis=-1, keepdims=True) + EPS)).astype(x.dtype)


def rope_tables(positions):
    inv = jnp.exp2(-jnp.arange(0, RET_DK, 2, dtype=jnp.float32) / RET_DK * jnp.log2(jnp.float32(ROPE_BASE)))
    ang = positions.astype(jnp.float32)[..., None] * inv
    return jnp.cos(ang)[:, :, None, :], jnp.sin(ang)[:, :, None, :]


def apply_rope(t, cos, sin):
    half = t.shape[-1] // 2
    t1, t2 = t[..., :half], t[..., half:]
    return jnp.concatenate([t1 * cos - t2 * sin, t1 * sin + t2 * cos], axis=-1)


def retention(q, k, v):
    b, s, h, dk = q.shape
    dv = v.shape[-1]
    n = s // RET_CHUNK
    C = RET_CHUNK
    log_gamma = jnp.log1p(-jnp.exp2(-5.0 - jnp.arange(h, dtype=jnp.float32)))

    def chunks(t):
        return t.astype(jnp.float32).reshape(b, n, C, h, t.shape[-1]).transpose(1, 0, 3, 2, 4)

    qc, kc, vc = chunks(q), chunks(k * (dk ** -0.5)), chunks(v)
    idx = jnp.arange(C, dtype=jnp.float32)
    rel = idx[:, None] - idx[None, :]
    decay = jnp.where(rel >= 0, jnp.exp(log_gamma[:, None, None] * jnp.maximum(rel, 0.0)), 0.0)
    q_decay = jnp.exp(log_gamma[:, None] * (idx[None, :] + 1.0))[None, :, :, None]
    k_decay = jnp.exp(log_gamma[:, None] * (C - 1.0 - idx[None, :]))[None, :, :, None]
    chunk_decay = jnp.exp(log_gamma * C)[None, :, None, None]

    def step(state, xs):
        qi, ki, vi = xs
        scores = jnp.einsum('bhqd,bhkd->bhqk', qi, ki) * decay
        intra = jnp.einsum('bhqk,bhkv->bhqv', scores, vi)
        cross = jnp.einsum('bhqd,bhdv->bhqv', qi, state) * q_decay
        new_state = state * chunk_decay + jnp.einsum('bhkd,bhkv->bhdv', ki * k_decay, vi)
        return new_state, intra + cross

    state0 = jnp.zeros((b, h, dk, dv), jnp.float32)
    _, out = lax.scan(step, state0, (qc, kc, vc))
    return out.transpose(1, 0, 3, 2, 4).reshape(b, s, h, dv)


def head_group_norm(y, gain):
    b, s, h, dv = y.shape
    mu = jnp.mean(y, axis=-1, keepdims=True)
    yc = y - mu
    yn = yc * lax.rsqrt(jnp.mean(yc * yc, axis=-1, keepdims=True) + EPS)
    return yn.reshape(b, s, h * dv) * gain


def causal_conv(x, w, bias):
    s = x.shape[1]
    xp = jnp.pad(x, ((0, 0), (CONV_WIDTH - 1, 0), (0, 0)))
    y = xp[:, 0:s] * w[0]
    for j in range(1, CONV_WIDTH):
        y = y + xp[:, j:j + s] * w[j]
    return y + bias


def rg_lru(x, w_a, b_a, w_x, b_x, lam):
    b, s, w = x.shape
    xf = x.astype(jnp.float32)
    xb = xf.reshape(b, s, LRU_BLOCKS, LRU_BW)
    r = jax.nn.sigmoid(jnp.einsum('bsnc,ncd->bsnd', xb, w_a.astype(jnp.float32)).reshape(b, s, w) + b_a)
    i = jax.nn.sigmoid(jnp.einsum('bsnc,ncd->bsnd', xb, w_x.astype(jnp.float32)).reshape(b, s, w) + b_x)
    log_a = -LRU_C * r * jax.nn.softplus(-lam.astype(jnp.float32))
    a = jnp.exp(log_a)
    u = jnp.sqrt(-jnp.expm1(2.0 * log_a)) * (i * xf)

    def combine(left, right):
        a1, b1 = left
        a2, b2 = right
        return a1 * a2, a2 * b1 + b2

    _, h = lax.associative_scan(combine, (a, u), axis=1)
    return h


def hybrid_mixer(hn, cos, sin, w_in, conv_w, conv_b, gate_a_w, gate_a_b, gate_x_w, gate_x_b,
                 lru_lambda, ret_norm_g, lru_norm_g, w_out):
    b, s, _ = hn.shape
    proj = hn @ w_in
    q, k, v, g, lru_gate, lru_x = jnp.split(proj, SPLITS, axis=-1)
    q = apply_rope(q.reshape(b, s, RET_HEADS, RET_DK), cos, sin)
    k = apply_rope(k.reshape(b, s, RET_HEADS, RET_DK), cos, sin)
    v = v.reshape(b, s, RET_HEADS, RET_DV)
    y_ret = head_group_norm(retention(q, k, v), ret_norm_g.astype(jnp.float32))
    y_ret = (jax.nn.silu(g.astype(jnp.float32)) * y_ret).astype(hn.dtype)
    xc = causal_conv(lru_x, conv_w, conv_b)
    h = rg_lru(xc, gate_a_w, gate_a_b, gate_x_w, gate_x_b, lru_lambda)
    h = h * lax.rsqrt(jnp.mean(h * h, axis=-1, keepdims=True) + EPS) * lru_norm_g.astype(jnp.float32)
    y_lru = (jax.nn.gelu(lru_gate.astype(jnp.float32)) * h).astype(hn.dtype)
    return jnp.concatenate([y_ret, y_lru], axis=-1) @ w_out


def swiglu(h, w_gate, w_up, w_down):
    return (jax.nn.silu(h @ w_gate) * (h @ w_up)) @ w_down


def moe_swiglu(h, w_router, w_gate, w_up, w_down):
    b, s, d = h.shape
    t = h.reshape(b * s, d)
    logits = (t @ w_router).astype(jnp.float32)
    top_val, top_idx = lax.top_k(logits, TOP_K)
    top_w = jax.nn.softmax(top_val, axis=-1)
    comb = jnp.sum(jax.nn.one_hot(top_idx, N_EXPERTS, dtype=jnp.float32) * top_w[..., None], axis=1)
    out = jnp.zeros((b * s, d), jnp.float32)
    for e in range(N_EXPERTS):
        he = jax.nn.silu(t @ w_gate[e]) * (t @ w_up[e])
        out = out + comb[:, e:e + 1] * (he @ w_down[e])
    return out.reshape(b, s, d).astype(h.dtype)


def setup_inputs(seed: int = 0) -> dict:
    key = jax.random.key(seed)
    ks = jax.random.split(key, 32)
    f32 = jnp.float32

    def nrm(k, shape, scale):
        return jax.random.normal(k, shape, f32) * scale

    x = nrm(ks[0], (BATCH, SEQ, D_MODEL), 1.0)
    c = nrm(ks[1], (BATCH, D_MODEL), 1.0)
    offsets = jax.random.randint(ks[2], (BATCH, 1), 0, 1024, dtype=jnp.int32)
    positions = offsets + jnp.arange(SEQ, dtype=jnp.int32)[None, :]
    ada_w = nrm(ks[3], (D_MODEL, N_MOD * D_MODEL), 0.5 * D_MODEL ** -0.5)
    ada_b = nrm(ks[4], (N_MOD * D_MODEL,), 0.02)
    ada_table = nrm(ks[5], (DEPTH, N_MOD, D_MODEL), 0.1)
    w_in = nrm(ks[6], (DEPTH, D_MODEL, IN_WIDTH), D_MODEL ** -0.5)
    conv_w = nrm(ks[7], (DEPTH, CONV_WIDTH, LRU_WIDTH), CONV_WIDTH ** -0.5)
    conv_b = nrm(ks[8], (DEPTH, LRU_WIDTH), 0.01)
    gate_a_w = nrm(ks[9], (DEPTH, LRU_BLOCKS, LRU_BW, LRU_BW), LRU_BW ** -0.5)
    gate_a_b = nrm(ks[10], (DEPTH, LRU_WIDTH), 0.01)
    gate_x_w = nrm(ks[11], (DEPTH, LRU_BLOCKS, LRU_BW, LRU_BW), LRU_BW ** -0.5)
    gate_x_b = nrm(ks[12], (DEPTH, LRU_WIDTH), 0.01)
    a_c = jax.random.uniform(ks[13], (DEPTH, LRU_WIDTH), f32, 0.9, 0.999)
    a0 = a_c ** (1.0 / LRU_C)
    lru_lambda = jnp.log(a0) - jnp.log1p(-a0)
    ret_norm_g = 1.0 + nrm(ks[14], (DEPTH, RET_V), 0.02)
    lru_norm_g = 1.0 + nrm(ks[15], (DEPTH, LRU_WIDTH), 0.02)
    w_out = nrm(ks[16], (DEPTH, MIX_WIDTH, D_MODEL), MIX_WIDTH ** -0.5)
    ffn_w_gate = nrm(ks[17], (N_DENSE, D_MODEL, D_FF), D_MODEL ** -0.5)
    ffn_w_up = nrm(ks[18], (N_DENSE, D_MODEL, D_FF), D_MODEL ** -0.5)
    ffn_w_down = nrm(ks[19], (N_DENSE, D_FF, D_MODEL), D_FF ** -0.5)
    router_w = nrm(ks[20], (N_MOE, D_MODEL, N_EXPERTS), D_MODEL ** -0.5)
    moe_w_gate = nrm(ks[21], (N_MOE, N_EXPERTS, D_MODEL, D_EXPERT), D_MODEL ** -0.5)
    moe_w_up = nrm(ks[22], (N_MOE, N_EXPERTS, D_MODEL, D_EXPERT), D_MODEL ** -0.5)
    moe_w_down = nrm(ks[23], (N_MOE, N_EXPERTS, D_EXPERT, D_MODEL), D_EXPERT ** -0.5)
    final_norm_g = 1.0 + nrm(ks[24], (D_MODEL,), 0.02)
    return {"x": x, "c": c, "positions": positions, "ada_w": ada_w, "ada_b": ada_b,
            "ada_table": ada_table, "w_in": w_in, "conv_w": conv_w, "conv_b": conv_b,
            "gate_a_w": gate_a_w, "gate_a_b": gate_a_b, "gate_x_w": gate_x_w, "gate_x_b": gate_x_b,
            "lru_lambda": lru_lambda, "ret_norm_g": ret_norm_g, "lru_norm_g": lru_norm_g,
            "w_out": w_out, "ffn_w_gate": ffn_w_gate, "ffn_w_up": ffn_w_up, "ffn_w_down": ffn_w_down,
            "router_w": router_w, "moe_w_gate": moe_w_gate, "moe_w_up": moe_w_up,
            "moe_w_down": moe_w_down, "final_norm_g": final_norm_g}


def reference(x, c, positions, ada_w, ada_b, ada_table, w_in, conv_w, conv_b, gate_a_w, gate_a_b,
              gate_x_w, gate_x_b, lru_lambda, ret_norm_g, lru_norm_g, w_out, ffn_w_gate, ffn_w_up,
              ffn_w_down, router_w, moe_w_gate, moe_w_up, moe_w_down, final_norm_g):
    b = x.shape[0]
    mod_base = (jax.nn.silu(c) @ ada_w + ada_b).reshape(b, N_MOD, D_MODEL)
    cos, sin = rope_tables(positions)
    for l in range(DEPTH):
        mod = mod_base + ada_table[l]
        shift1, scale1, gate1, shift2, scale2, gate2 = [mod[:, j, None, :] for j in range(N_MOD)]
        hn = rms_norm(x) * (1.0 + scale1) + shift1
        y = hybrid_mixer(hn, cos, sin, w_in[l], conv_w[l], conv_b[l], gate_a_w[l], gate_a_b[l],
                         gate_x_w[l], gate_x_b[l], lru_lambda[l], ret_norm_g[l], lru_norm_g[l], w_out[l])
        x = x + gate1 * y
        hn = rms_norm(x) * (1.0 + scale2) + shift2
        if l % 2 == 0:
            y = swiglu(hn, ffn_w_gate[l // 2], ffn_w_up[l // 2], ffn_w_down[l // 2])
        else:
            y = moe_swiglu(hn, router_w[l // 2], moe_w_gate[l // 2], moe_w_up[l // 2], moe_w_down[l // 2])
        x = x + gate2 * y
    return rms_norm(x) * final_norm_g
```

```python
import contextlib
import numpy as np
import concourse.bass as bass
import concourse.mybir as mybir
from concourse.bass_utils import run_bass_kernel_spmd

ALU = mybir.AluOpType
AF = mybir.ActivationFunctionType
F32, BF16, I32 = mybir.dt.float32, mybir.dt.bfloat16, mybir.dt.int32
GW = 256
EPS = 1e-6
SAME_ENG_SYNC = True

FULL = dict(D=4096, H=8, LW=2048, FF=8192, E=8, DE=1792, L=4, SEQ=8192, TT=512, B=2)


class Buf:
    __slots__ = ("name", "w", "rs", "cnt", "sem")

    def __init__(self, name):
        self.name = name; self.w = None; self.rs = []; self.cnt = 0; self.sem = None


class Sched:
    ENGS = ("pe", "act", "dve", "pool", "sp")

    def __init__(self):
        self.q = {e: [] for e in self.ENGS}
        self.dmabufs = []

    def _deps(self, rd, wr):
        deps = []
        for b in rd:
            if b.w is not None:
                deps.append(b.w)
        for b in wr:
            if b.w is not None:
                deps.append(b.w)
            deps.extend(b.rs)
        return deps

    def op(self, eng, fn, rd=(), wr=()):
        deps = self._deps(rd, wr)
        ev = ("e", eng, len(self.q[eng]))
        self.q[eng].append([fn, deps, False, None])
        for b in rd:
            b.rs.append(ev)
        for b in wr:
            b.w = ev; b.rs = []
        return ev

    def dma(self, eng, fn, src, dst, n=1):
        deps = self._deps([src], [dst])
        if dst.sem is None:
            dst.sem = True; self.dmabufs.append(dst)
        dst.cnt += 16 * n
        ev = ("d", dst, dst.cnt)
        self.q[eng].append([fn, deps, False, dst])
        src.rs.append(ev)
        dst.w = ev; dst.rs = []
        return ev

    def emit(self, nc, stack):
        for e in self.ENGS:
            for ins in self.q[e]:
                for d in ins[1]:
                    if d[0] == "e":
                        if d[1] == "pe" and e == "pe":
                            continue
                        if d[1] == e and not SAME_ENG_SYNC:
                            continue
                        self.q[d[1]][d[2]][2] = True
        cum = {}
        for e in self.ENGS:
            c = 0; arr = []
            for ins in self.q[e]:
                if ins[2]:
                    c += 1
                arr.append(c)
            cum[e] = arr
        esem = {e: stack.enter_context(nc.semaphore("es_" + e)) for e in self.ENGS}
        for b in self.dmabufs:
            b.sem = stack.enter_context(nc.semaphore("ds_" + b.name))
        engobj = {"pe": "tensor", "act": "scalar", "dve": "vector", "pool": "gpsimd", "sp": "sync"}
        block = stack.enter_context(nc.Block())
        finals = [(b.sem, b.cnt) for b in self.dmabufs]

        def run(e):
            def body(eng):
                waited = {}
                for idx, ins in enumerate(self.q[e]):
                    need = {}
                    for d in ins[1]:
                        if d[0] == "e":
                            if d[1] == "pe" and e == "pe":
                                continue
                            if d[1] == e and not SAME_ENG_SYNC:
                                continue
                            key = ("e", d[1]); sem = esem[d[1]]; val = cum[d[1]][d[2]]
                        else:
                            key = ("d", id(d[1])); sem = d[1].sem; val = d[2]
                        if val > need.get(key, (None, 0))[1]:
                            need[key] = (sem, val)
                    for key, (sem, val) in need.items():
                        if waited.get(key, 0) >= val:
                            continue
                        waited[key] = val
                        eng.wait_ge(sem, val)
                    if ins[3] is not None:
                        ins[0](eng, ins[3].sem)
                    else:
                        r = ins[0](eng)
                        if ins[2]:
                            r.then_inc(esem[e], 1)
                if e == "sp":
                    for sem, cnt in finals:
                        eng.wait_ge(sem, cnt)
            return body
        for e in self.ENGS:
            getattr(block, engobj[e])(run(e))


def build(cfg):
    D, H, LW, FF, E, DE, L, SEQ, TT = (cfg[k] for k in ("D", "H", "LW", "FF", "E", "DE", "L", "SEQ", "TT"))
    KD = D // 128; Vw = H * 256; NBL = LW // 128; MIX = Vw + LW; MKC = MIX // 128
    NT = SEQ // TT; MS = TT // 128; NCH = SEQ // 128
    NBI = (2 * H * 128 + 2 * Vw + 2 * LW) // GW
    DG = D // GW
    FH = FF // 128 // 2
    NQ = 4; EQ = E // NQ; DEC = DE // 128; QC = EQ * DEC
    nc = bass.Bass("TRN2", target_bir_lowering=False)
    S = Sched()
    st = contextlib.ExitStack()

    def din(name, shape, dt=F32):
        return nc.dram_tensor(name, list(shape), dt, kind="ExternalInput").ap()

    def dint(name, shape, dt):
        return nc.dram_tensor(name, list(shape), dt, kind="Internal").ap()

    xT_in = din("xT", [KD, 128, SEQ])
    outT = nc.dram_tensor("outT", [KD, 128, SEQ], F32, kind="ExternalOutput").ap()
    xs = dint("xs", [KD, 128, SEQ], F32)
    cT = din("cT", [128, KD]); pos = din("pos", [128, NCH], I32)
    ada_w = din("ada_w", [6 * KD, 128, KD, 128]); ada_b = din("ada_b", [128, 6 * KD]); ada_tab = din("ada_tab", [128, L, 6 * KD])
    convw = din("convw", [128, L, 4, NBL])
    lp = {n: din(n, [128, L, NBL]) for n in ("convb", "gab", "gxb", "lam", "lng")}
    rng = din("rng", [128, L, 2 * H]); fng = din("fng", [128, KD])
    gaw = din("gaw", [128, L, NBL, 128]); gxw = din("gxw", [128, L, NBL, 128])
    router = din("router", [128, max(L // 2, 1), KD, E])
    maskT_d = din("maskT", [128, H, 128]); qdec_d = din("qdec", [128, H, 128]); kdec_d = din("kdec", [128, H]); gC_d = din("gC", [128, H])
    invf_d = din("invf", [128, 64]); ident_d = din("ident", [128, 128])

    wspec = {}
    for l in range(L):
        wspec[f"win{l}"] = [NBI, 128, KD, GW]
        wspec[f"wout{l}"] = [DG, 128, MKC, GW]
        if l % 2 == 0:
            wspec[f"wg{l}"] = [FF // GW, 128, KD, GW]; wspec[f"wu{l}"] = [FF // GW, 128, KD, GW]
            wspec[f"wd{l}"] = [2 * DG, 128, FH, GW]
        else:
            wspec[f"wg{l}"] = [E * DE // GW, 128, KD, GW]; wspec[f"wu{l}"] = [E * DE // GW, 128, KD, GW]
            wspec[f"wd{l}"] = [NQ * DG, 128, QC, GW]
    wf, wb, wbuf = {}, {}, {}
    for n, shp in wspec.items():
        wf[n] = din(n, shp); wb[n] = dint(n + "_b", shp, BF16); wbuf[n] = Buf(n)
    ext = Buf("ext")

    def sb(name, shape, dt=F32):
        return st.enter_context(nc.sbuf_tensor("sb_" + name, list(shape), dt))

    def ps(name):
        return st.enter_context(nc.psum_tensor(name, [128, 512], F32))

    NWT = 3
    wt = [sb(f"wt{i}", [128, 32, GW], BF16) for i in range(NWT)]; wt_b = [Buf(f"wt{i}") for i in range(NWT)]
    hnT = sb("hnT", [128, KD, TT], BF16); hn_b = Buf("hnT")
    yT = sb("yT", [128, max(MKC, FH, QC), TT], BF16); y_b = Buf("yT")
    NXC = 3
    xc = [sb(f"xc{i}", [128, TT]) for i in range(NXC)]; xc_b = [Buf(f"xc{i}") for i in range(NXC)]
    NTMP = 7
    tmp = [sb(f"tmp{i}", [128, TT]) for i in range(NTMP)]; tmp_b = [Buf(f"tmp{i}") for i in range(NTMP)]
    sqb = [sb(f"sq{i}", [128, TT], BF16) for i in range(2)]; sq_b = [Buf(f"sq{i}") for i in range(2)]
    rstd = sb("rstd", [128, TT]); rstd_b = Buf("rstd")
    cosT = sb("cosT", [128, MS, 64]); sinT = sb("sinT", [128, MS, 64]); rope_b = Buf("rope")
    mod = sb("mod", [128, 6 * KD]); modb = sb("modb", [128, 6 * KD]); mod_b = Buf("mod"); modb_b = Buf("modb")
    tabs = sb("tabs", [128, 6 * KD]); tabs_b = Buf("tabs")
    ident = sb("ident", [128, 128], BF16); identf = sb("identf", [128, 128]); ones = sb("ones", [128, 128], BF16); con_b = Buf("consts")
    maskT = sb("maskT", [128, H, 128]); qdec = sb("qdec", [128, H, 128]); kdec = sb("kdec", [128, H]); gC = sb("gC", [128, H])
    Sst = sb("Sst", [128, H, 256]); Sbf = sb("Sbf", [128, H, 256], BF16); S_b = [Buf(f"S{h}") for h in range(H)]
    hst = sb("hst", [128, NBL]); h_b = [Buf(f"h{n}") for n in range(NBL)]
    halo = sb("halo", [128, NBL, 3]); halo_b = [Buf(f"halo{n}") for n in range(NBL)]
    lxt = [sb(f"lxt{i}", [128, 3 + TT]) for i in range(2)]; lxt_b = [Buf(f"lxt{i}") for i in range(2)]
    lpar = {n: sb("s_" + n, [128, L, NBL]) for n in lp}; cw = sb("cw", [128, L, 4, NBL]); cneg = sb("cneg", [128, L, NBL]); cneg2 = sb("cneg2", [128, L, NBL]); lpar_b = Buf("lpar")
    gwa = sb("gwa", [128, NBL, 128], BF16); gwx = sb("gwx", [128, NBL, 128], BF16); gw_b = Buf("gw")
    rngs = sb("rngs", [128, L, 2 * H]); rng_b = con_b
    fngs = sb("fngs", [128, KD])
    rw = sb("rw", [128, KD, E]); rw_b = Buf("rw")
    qks = sb("qks", [128, 2, 128], BF16); qks_b = Buf("qks")
    kds = sb("kds", [128, MS, 128], BF16); kds_b = Buf("kds")
    qTs = sb("qTs", [128, 3, TT], BF16); qT_b = Buf("qTs")
    vs = sb("vs", [128, MS, 256], BF16); vs_b = Buf("vs")
    gs = sb("gs", [128, MS, 256], BF16); gs_b = Buf("gs")
    sTs = [sb(f"sT{i}", [128, 128], BF16) for i in range(2)]; sT_b = [Buf(f"sT{i}") for i in range(2)]
    yrs = [sb(f"yr{i}", [128, 256]) for i in range(2)]; yr_b = [Buf(f"yr{i}") for i in range(2)]
    yrb = [sb(f"yrb{i}", [128, 256], BF16) for i in range(2)]; yrb_b = [Buf(f"yrb{i}") for i in range(2)]
    bst = sb("bst", [128, 8]); bst_b = Buf("bst")
    comb = sb("comb", [128, MS, E]); comb_b = Buf("comb"); combT = sb("combT", [E, TT]); combT_b = Buf("combT")
    sel = sb("sel", [E, E, 128]); combB = sb("combB", [128, TT]); combB_b = Buf("combB"); sel_b = Buf("sel")
    small = sb("small", [128, 64]); small_b = Buf("small")
    cs = sb("cs", [128, KD]); cs_b = Buf("cs")
    pst = [ps(f"ps{i}") for i in range(8)]; ps_b = [Buf(f"ps{i}") for i in range(8)]
    psrr = [0]

    def nps(lo=0, hi=6):
        i = lo + psrr[0] % (hi - lo); psrr[0] += 1
        return pst[i], ps_b[i]
    trr = [0]

    def ntmp():
        i = trr[0] % NTMP; trr[0] += 1
        return tmp[i], tmp_b[i]
    xrr = [0]

    def nxc():
        i = xrr[0] % NXC; xrr[0] += 1
        return xc[i], xc_b[i]

    def load(dst_ap, src_ap, dbuf, sbuf=ext, eng="sp"):
        S.dma(eng, lambda e, sem: e.dma_start(out=dst_ap, in_=src_ap).then_inc(sem, 16), sbuf, dbuf)

    def dve(fn, rd, wr):
        S.op("dve", fn, rd, wr)

    def act(fn, rd, wr):
        S.op("act", fn, rd, wr)

    def pe(fn, rd, wr):
        S.op("pe", fn, rd, wr)

    def cast_weight(n):
        tot = int(np.prod(wspec[n])); rows = tot // 2048
        src = wf[n].rearrange("a p k c -> (a p k c)").rearrange("(r c) -> r c", c=2048)
        dst = wb[n].rearrange("a p k c -> (a p k c)").rearrange("(r c) -> r c", c=2048)
        step = 4096
        for r0 in range(0, rows, step):
            r1 = min(rows, r0 + step)
            S.dma("pool", lambda e, sem, r0=r0, r1=r1: e.dma_start(out=dst[r0:r1, :], in_=src[r0:r1, :]).then_inc(sem, 16), ext, wbuf[n])
    for l in range(L):
        for n in (f"win{l}", f"wout{l}", f"wg{l}", f"wu{l}", f"wd{l}"):
            cast_weight(n)

    load(identf[:], ident_d, con_b)
    dve(lambda e: e.tensor_copy(out=ident[:], in_=identf[:]), [con_b], [con_b])
    dve(lambda e: e.memset(ones[:], 1.0), [], [con_b])
    for t_, d_ in ((maskT, maskT_d), (qdec, qdec_d), (kdec, kdec_d), (gC, gC_d), (cw, convw), (fngs, fng), (rngs, rng)):
        load(t_[:], d_, con_b)
    for n in lp:
        load(lpar[n][:], lp[n], lpar_b)
    act(lambda e: e.activation(out=cneg[:], in_=lpar["lam"][:], func=AF.Exp, scale=-1.0), [lpar_b], [lpar_b])
    act(lambda e: e.activation(out=cneg[:], in_=cneg[:], func=AF.Ln, bias=1.0, scale=1.0), [lpar_b], [lpar_b])
    dve(lambda e: e.tensor_scalar(out=cneg2[:], in0=cneg[:], scalar1=-16.0, scalar2=None, op0=ALU.mult), [lpar_b], [lpar_b])
    dve(lambda e: e.tensor_scalar(out=cneg[:], in0=cneg[:], scalar1=-8.0, scalar2=None, op0=ALU.mult), [lpar_b], [lpar_b])
    dve(lambda e: e.memset(Sst[:], 0.0), [], S_b)
    dve(lambda e: e.memset(Sbf[:], 0.0), [], S_b)
    dve(lambda e: e.memset(hst[:], 0.0), [], h_b)
    dve(lambda e: e.memset(halo[:], 0.0), [], halo_b)
    dve(lambda e: e.memset(sel[:], 0.0), [], [sel_b])
    dve(lambda e: e.tensor_copy(out=sel[:, :, :], in_=identf[0:E, 0:E].unsqueeze(2).broadcast_to([E, E, 128])), [con_b, sel_b], [sel_b])

    posi = st.enter_context(nc.sbuf_tensor("sb_posi", [128, NCH], I32)); posf = sb("posf", [128, NCH]); invf = sb("invf", [128, 64])
    angb = sb("angb", [128, MS, 64]); angn = sb("angn", [128, MS, 64]); angi = st.enter_context(nc.sbuf_tensor("sb_angi", [128, MS, 64], I32))
    load(posi[:], pos, rope_b); load(invf[:], invf_d, rope_b)
    dve(lambda e: e.tensor_copy(out=posf[:], in_=posi[:]), [rope_b], [rope_b])
    TWO_PI = 2.0 * np.pi; C1 = 6.28125; C2 = TWO_PI - C1

    def rope_tile(t):
        for table, shift in ((sinT, 0.0), (cosT, 0.5 * np.pi)):
            dve(lambda e: e.tensor_tensor(out=angb[:], in0=posf[:, t * MS:(t + 1) * MS].unsqueeze(2).broadcast_to([128, MS, 64]),
                                          in1=invf[:].unsqueeze(1).broadcast_to([128, MS, 64]), op=ALU.mult), [rope_b], [rope_b])
            if shift:
                dve(lambda e, s_=shift: e.tensor_scalar(out=angb[:], in0=angb[:], scalar1=float(s_), scalar2=None, op0=ALU.add), [rope_b], [rope_b])
            dve(lambda e: e.tensor_scalar(out=angn[:], in0=angb[:], scalar1=float(1.0 / TWO_PI), scalar2=None, op0=ALU.mult), [rope_b], [rope_b])
            dve(lambda e: e.tensor_copy(out=angi[:], in_=angn[:]), [rope_b], [rope_b])
            dve(lambda e: e.tensor_copy(out=angn[:], in_=angi[:]), [rope_b], [rope_b])
            dve(lambda e: e.scalar_tensor_tensor(out=angb[:], in0=angn[:], scalar=float(-C1), in1=angb[:], op0=ALU.mult, op1=ALU.add), [rope_b], [rope_b])
            dve(lambda e: e.scalar_tensor_tensor(out=angb[:], in0=angn[:], scalar=float(-C2), in1=angb[:], op0=ALU.mult, op1=ALU.add), [rope_b], [rope_b])
            dve(lambda e: e.tensor_scalar(out=angn[:], in0=angb[:], scalar1=float(np.pi), scalar2=float(-TWO_PI), op0=ALU.is_gt, op1=ALU.mult), [rope_b], [rope_b])
            dve(lambda e: e.tensor_tensor(out=angb[:], in0=angb[:], in1=angn[:], op=ALU.add), [rope_b], [rope_b])
            dve(lambda e: e.tensor_scalar(out=angn[:], in0=angb[:], scalar1=float(-np.pi), scalar2=float(TWO_PI), op0=ALU.is_lt, op1=ALU.mult), [rope_b], [rope_b])
            dve(lambda e: e.tensor_tensor(out=angb[:], in0=angb[:], in1=angn[:], op=ALU.add), [rope_b], [rope_b])
            act(lambda e, t_=table: e.activation(out=t_[:], in_=angb[:], func=AF.Sin), [rope_b], [rope_b])

    load(cs[:], cT, cs_b)
    act(lambda e: e.activation(out=cs[:], in_=cs[:], func=AF.Silu), [cs_b], [cs_b])
    load(modb[:], ada_b, modb_b)
    yTf = yT[:, 0:KD, 0:256].bitcast(F32) if TT >= 256 else None
    adaw_t = [yTf[:, :, 0:128]]; adaw_b = [y_b]
    for g in range(6 * KD):
        i = 0
        load(adaw_t[i], ada_w[g], adaw_b[i])
        p_, pb_ = nps(6, 8)
        for k in range(KD):
            pe(lambda e, p_=p_, i=i, k=k: e.matmul(p_[:, 0:1], lhsT=adaw_t[i][:, k, :], rhs=cs[:, k:k + 1], start=(k == 0), stop=(k == KD - 1)),
               [adaw_b[i], cs_b], [pb_])
        dve(lambda e, p_=p_, g=g: e.tensor_tensor(out=modb[:, g:g + 1], in0=p_[:, 0:1], in1=modb[:, g:g + 1], op=ALU.add), [pb_, modb_b], [modb_b])

    xs_b = Buf("xs")
    for k in range(KD):
        S.dma("sp", lambda e, sem, k=k: e.dma_start(out=xs[k], in_=xT_in[k]).then_inc(sem, 16), ext, xs_b)

    wrr = [0]

    def wload(name, idx, kc):
        i = wrr[0] % NWT; wrr[0] += 1
        S.dma("sp", lambda e, sem, i=i: e.dma_start(out=wt[i][:, 0:kc, :], in_=wb[name][idx]).then_inc(sem, 16), wbuf[name], wt_b[i])
        return wt[i], wt_b[i]

    def norm_to_hn(t, a_off, s_off, src=None, want_f32=False):
        t0 = t * TT
        pst_, pstb = nps(6, 8)
        for k in range(KD):
            x_, xb = nxc()
            load(x_[:], xs[k, :, t0:t0 + TT], xb, xs_b)
            i = k % 2
            act(lambda e, x_=x_, i=i: e.activation(out=sqb[i][:], in_=x_[:], func=AF.Square), [xb], [sq_b[i]])
            pe(lambda e, i=i, k=k: e.matmul(pst_[:, 0:TT], lhsT=ones[:], rhs=sqb[i][:], start=(k == 0), stop=(k == KD - 1)), [sq_b[i], con_b], [pstb])
        act(lambda e: e.activation(out=rstd[:], in_=pst_[:, 0:TT], func=AF.Sqrt, bias=float(EPS), scale=float(1.0 / D)), [pstb], [rstd_b])
        dve(lambda e: e.reciprocal(out=rstd[:], in_=rstd[:]), [rstd_b], [rstd_b])
        for k in range(KD):
            x_, xb = nxc()
            load(x_[:], xs[k, :, t0:t0 + TT], xb, xs_b)
            t_, tb = ntmp()
            dve(lambda e, x_=x_, t_=t_, k=k: e.scalar_tensor_tensor(out=t_[:], in0=x_[:], scalar=mod[:, a_off + k:a_off + k + 1], in1=rstd[:],
                                                                    op0=ALU.mult, op1=ALU.mult), [xb, rstd_b, mod_b], [tb])
            act(lambda e, t_=t_, k=k: e.activation(out=hnT[:, k, :], in_=t_[:], func=AF.Identity, bias=mod[:, s_off + k:s_off + k + 1], scale=1.0),
                [tb, mod_b], [hn_b])

    def resid_update(t, dch, p_, pb_, g_off):
        t0 = t * TT
        x_, xb = nxc()
        load(x_[:], xs[dch, :, t0:t0 + TT], xb, xs_b)
        dve(lambda e: e.scalar_tensor_tensor(out=x_[:], in0=p_[:, 0:TT], scalar=mod[:, g_off + dch:g_off + dch + 1], in1=x_[:], op0=ALU.mult, op1=ALU.add),
            [pb_, xb, mod_b], [xb])
        S.dma("pool", lambda e, sem: e.dma_start(out=xs[dch, :, t0:t0 + TT], in_=x_[:]).then_inc(sem, 16), xb, xs_b)

    def gemm_fm(name, idx, kc, src, srcb, j, p_, pb_):
        pass

    for l in range(L):
        moe = (l % 2 == 1)
        load(tabs[:], ada_tab[:, l, :], tabs_b)
        dve(lambda e, l=l: e.tensor_tensor(out=mod[:], in0=modb[:], in1=tabs[:], op=ALU.add), [modb_b, tabs_b], [mod_b])
        for j in (1, 4):
            dve(lambda e, j=j: e.tensor_scalar(out=mod[:, j * KD:(j + 1) * KD], in0=mod[:, j * KD:(j + 1) * KD], scalar1=1.0, scalar2=None, op0=ALU.add), [mod_b], [mod_b])
        load(gwa[:], gaw[:, l], gw_b, eng="pool")
        load(gwx[:], gxw[:, l], gw_b, eng="pool")
        if moe:
            load(rw[:], router[:, l // 2], rw_b)
        dve(lambda e: e.memset(Sst[:], 0.0), [], S_b)
        dve(lambda e: e.memset(Sbf[:], 0.0), [], S_b)
        dve(lambda e: e.memset(hst[:], 0.0), [], h_b)
        dve(lambda e: e.memset(halo[:], 0.0), [], halo_b)

        for t in range(NT):
            t0 = t * TT
            rope_tile(t)
            norm_to_hn(t, 1 * KD, 0 * KD)
            for h in range(H):
                w_, wb_ = wload(f"win{l}", 3 * h, KD)
                for m in range(MS):
                    p_, pb_ = nps()
                    for k in range(KD):
                        pe(lambda e, p_=p_, w_=w_, m=m, k=k: e.matmul(p_[:, 0:GW], lhsT=hnT[:, k, m * 128:(m + 1) * 128], rhs=w_[:, k, :], start=(k == 0), stop=(k == KD - 1)),
                           [hn_b, wb_], [pb_])
                    ci = m
                    pv = p_[:, 0:GW].rearrange("p (a b c) -> p a b c", a=2, b=2)
                    t1, tb1 = ntmp(); t2, tb2 = ntmp()
                    cosb = cosT[:, ci, :].unsqueeze(1).broadcast_to([128, 2, 64]); sinb = sinT[:, ci, :].unsqueeze(1).broadcast_to([128, 2, 64])
                    a1 = t1[:, 0:128].rearrange("p (a c) -> p a c", a=2); a2 = t1[:, 128:256].rearrange("p (a c) -> p a c", a=2)
                    b1 = t2[:, 0:128].rearrange("p (a c) -> p a c", a=2); b2 = t2[:, 128:256].rearrange("p (a c) -> p a c", a=2)
                    qv = qks[:].rearrange("p a (b c) -> p a b c", b=2)
                    dve(lambda e, pv=pv, a1=a1, cosb=cosb: e.tensor_tensor(out=a1, in0=pv[:, :, 0, :], in1=cosb, op=ALU.mult), [pb_, rope_b], [tb1])
                    dve(lambda e, pv=pv, a2=a2, sinb=sinb: e.tensor_tensor(out=a2, in0=pv[:, :, 1, :], in1=sinb, op=ALU.mult), [pb_, rope_b], [tb1])
                    dve(lambda e, pv=pv, b1=b1, sinb=sinb: e.tensor_tensor(out=b1, in0=pv[:, :, 0, :], in1=sinb, op=ALU.mult), [pb_, rope_b], [tb2])
                    dve(lambda e, pv=pv, b2=b2, cosb=cosb: e.tensor_tensor(out=b2, in0=pv[:, :, 1, :], in1=cosb, op=ALU.mult), [pb_, rope_b], [tb2])
                    dve(lambda e, qv=qv, a1=a1, a2=a2: e.tensor_tensor(out=qv[:, :, 0, :], in0=a1, in1=a2, op=ALU.subtract), [tb1], [qks_b])
                    dve(lambda e, qv=qv, b1=b1, b2=b2: e.tensor_tensor(out=qv[:, :, 1, :], in0=b1, in1=b2, op=ALU.add), [tb2], [qks_b])
                    dve(lambda e, m=m, h=h: e.tensor_scalar(out=kds[:, m, :], in0=qks[:, 1, :], scalar1=kdec[:, h:h + 1], scalar2=None, op0=ALU.mult), [qks_b, con_b], [kds_b])
                    pt, ptb = nps(6, 8)
                    ptv = pt[:].bitcast(BF16)
                    pe(lambda e, ptv=ptv: e.transpose(out=ptv[:, 0:128], in_=qks[:, 0, :], identity=ident[:]), [qks_b, con_b], [ptb])
                    pe(lambda e, ptv=ptv: e.transpose(out=ptv[:, 128:256], in_=qks[:, 1, :], identity=ident[:]), [qks_b, con_b], [ptb])
                    act(lambda e, ptv=ptv, m=m: e.activation(out=qTs[:, 0, m * 128:(m + 1) * 128], in_=ptv[:, 0:128], func=AF.Copy), [ptb], [qT_b])
                    dve(lambda e, ptv=ptv, m=m, h=h: e.tensor_tensor(out=qTs[:, 1, m * 128:(m + 1) * 128], in0=ptv[:, 0:128], in1=qdec[:, h, :], op=ALU.mult), [ptb, con_b], [qT_b])
                    act(lambda e, ptv=ptv, m=m: e.activation(out=qTs[:, 2, m * 128:(m + 1) * 128], in_=ptv[:, 128:256], func=AF.Copy), [ptb], [qT_b])
                w_, wb_ = wload(f"win{l}", 3 * h + 1, KD)
                for m in range(MS):
                    p_, pb_ = nps()
                    for k in range(KD):
                        pe(lambda e, p_=p_, w_=w_, m=m, k=k: e.matmul(p_[:, 0:GW], lhsT=hnT[:, k, m * 128:(m + 1) * 128], rhs=w_[:, k, :], start=(k == 0), stop=(k == KD - 1)),
                           [hn_b, wb_], [pb_])
                    act(lambda e, p_=p_, m=m: e.activation(out=vs[:, m, :], in_=p_[:, 0:GW], func=AF.Copy), [pb_], [vs_b])
                w_, wb_ = wload(f"win{l}", 3 * h + 2, KD)
                for m in range(MS):
                    p_, pb_ = nps()
                    for k in range(KD):
                        pe(lambda e, p_=p_, w_=w_, m=m, k=k: e.matmul(p_[:, 0:GW], lhsT=hnT[:, k, m * 128:(m + 1) * 128], rhs=w_[:, k, :], start=(k == 0), stop=(k == KD - 1)),
                           [hn_b, wb_], [pb_])
                    act(lambda e, p_=p_, m=m: e.activation(out=gs[:, m, :], in_=p_[:, 0:GW], func=AF.Silu), [pb_], [gs_b])
                for m in range(MS):
                    msl = slice(m * 128, (m + 1) * 128)
                    i2 = m % 2
                    pS, pSb = nps()
                    pe(lambda e, pS=pS, msl=msl: e.matmul(pS[:, 0:128], lhsT=qTs[:, 2, msl], rhs=qTs[:, 0, msl], start=True, stop=True), [qT_b], [pSb])
                    dve(lambda e, pS=pS, i2=i2, h=h: e.tensor_tensor(out=sTs[i2][:], in0=pS[:, 0:128], in1=maskT[:, h, :], op=ALU.mult), [pSb, con_b], [sT_b[i2]])
                    pO, pOb = nps()
                    pe(lambda e, pO=pO, i2=i2, m=m: e.matmul(pO[:, 0:256], lhsT=sTs[i2][:], rhs=vs[:, m, :], start=True, stop=False), [sT_b[i2], vs_b], [pOb])
                    pe(lambda e, pO=pO, msl=msl, h=h: e.matmul(pO[:, 0:256], lhsT=qTs[:, 1, msl], rhs=Sbf[:, h, :], start=False, stop=True), [qT_b, S_b[h]], [pOb])
                    pD, pDb = nps()
                    pe(lambda e, pD=pD, m=m: e.matmul(pD[:, 0:256], lhsT=kds[:, m, :], rhs=vs[:, m, :], start=True, stop=True), [kds_b, vs_b], [pDb])
                    dve(lambda e, pD=pD, h=h: e.scalar_tensor_tensor(out=Sst[:, h, :], in0=Sst[:, h, :], scalar=gC[:, h:h + 1], in1=pD[:, 0:256], op0=ALU.mult, op1=ALU.add),
                        [pDb, con_b, S_b[h]], [S_b[h]])
                    act(lambda e, h=h: e.activation(out=Sbf[:, h, :], in_=Sst[:, h, :], func=AF.Copy), [S_b[h]], [S_b[h]])
                    yr, yrb_ = yrs[i2], yr_b[i2]
                    dve(lambda e, pO=pO: e.bn_stats(out=bst[:, 0:6], in_=pO[:, 0:256]), [pOb], [bst_b])
                    dve(lambda e: e.bn_aggr(out=small[:, 0:2], in_=bst[:, 0:6]), [bst_b], [small_b])
                    act(lambda e: e.activation(out=small[:, 2:3], in_=small[:, 1:2], func=AF.Sqrt, bias=float(EPS), scale=1.0), [small_b], [small_b])
                    dve(lambda e: e.reciprocal(out=small[:, 2:3], in_=small[:, 2:3]), [small_b], [small_b])
                    dve(lambda e, pO=pO, yr=yr: e.tensor_scalar(out=yr[:], in0=pO[:, 0:256], scalar1=small[:, 0:1], scalar2=small[:, 2:3], op0=ALU.subtract, op1=ALU.mult),
                        [pOb, small_b], [yrb_])
                    dve(lambda e, yr=yr, i2=i2, m=m: e.tensor_tensor(out=yrb[i2][:], in0=yr[:], in1=gs[:, m, :], op=ALU.mult), [yrb_, gs_b], [yrb_b[i2]])
                    pt, ptb = nps(6, 8)
                    ptv = pt[:].bitcast(BF16)
                    for jj in range(2):
                        pe(lambda e, ptv=ptv, i2=i2, jj=jj: e.transpose(out=ptv[:, jj * 128:(jj + 1) * 128], in_=yrb[i2][:, jj * 128:(jj + 1) * 128], identity=ident[:]),
                           [yrb_b[i2], con_b], [ptb])
                    for jj in range(2):
                        act(lambda e, ptv=ptv, jj=jj, h=h, msl=msl: e.activation(out=yT[:, 2 * h + jj, msl], in_=ptv[:, jj * 128:(jj + 1) * 128], func=AF.Identity, scale=rngs[:, l, 2 * h + jj:2 * h + jj + 1]), [ptb, con_b], [y_b])
            ssq, ssqb = pst[7], ps_b[7]
            for n in range(NBL):
                w_, wb_ = wload(f"win{l}", 3 * H + n, KD)
                pG, pGb = nps(); pX, pXb = nps()
                for k in range(KD):
                    pe(lambda e, pG=pG, w_=w_, k=k: e.matmul(pG[:, 0:TT], lhsT=w_[:, k, 0:128], rhs=hnT[:, k, :], start=(k == 0), stop=(k == KD - 1)), [hn_b, wb_], [pGb])
                for k in range(KD):
                    pe(lambda e, pX=pX, w_=w_, k=k: e.matmul(pX[:, 0:TT], lhsT=w_[:, k, 128:256], rhs=hnT[:, k, :], start=(k == 0), stop=(k == KD - 1)), [hn_b, wb_], [pXb])
                tg, tgb = ntmp(); tu, tub = ntmp()
                act(lambda e, pG=pG, tg=tg: e.activation(out=tg[:], in_=pG[:, 0:TT], func=AF.Square), [pGb], [tgb])
                dve(lambda e, tg=tg: e.tensor_scalar(out=tg[:], in0=tg[:], scalar1=0.044715, scalar2=1.0, op0=ALU.mult, op1=ALU.add), [tgb], [tgb])
                dve(lambda e, tg=tg, pG=pG: e.tensor_tensor(out=tg[:], in0=tg[:], in1=pG[:, 0:TT], op=ALU.mult), [tgb, pGb], [tgb])
                act(lambda e, tg=tg: e.activation(out=tg[:], in_=tg[:], func=AF.Sigmoid, scale=float(2.0 * np.sqrt(2.0 / np.pi))), [tgb], [tgb])
                dve(lambda e, tg=tg, pG=pG: e.tensor_tensor(out=tg[:], in0=tg[:], in1=pG[:, 0:TT], op=ALU.mult), [tgb, pGb], [tgb])
                lx_, lxb_ = lxt[n % 2], lxt_b[n % 2]
                dve(lambda e, lx_=lx_, n=n: e.tensor_copy(out=lx_[:, 0:3], in_=halo[:, n, :]), [halo_b[n]], [lxb_])
                act(lambda e, pX=pX, lx_=lx_: e.activation(out=lx_[:, 3:3 + TT], in_=pX[:, 0:TT], func=AF.Copy), [pXb], [lxb_])
                xcv, xcvb = ntmp()
                act(lambda e, xcv=xcv, lx_=lx_, n=n, l=l: e.activation(out=xcv[:], in_=lx_[:, 3:3 + TT], func=AF.Identity, bias=lpar["convb"][:, l, n:n + 1], scale=cw[:, l, 3, n:n + 1]),
                    [lxb_, lpar_b, con_b], [xcvb])
                for j in range(3):
                    dve(lambda e, xcv=xcv, lx_=lx_, n=n, j=j, l=l: e.scalar_tensor_tensor(out=xcv[:], in0=lx_[:, j:j + TT], scalar=cw[:, l, j, n:n + 1], in1=xcv[:], op0=ALU.mult, op1=ALU.add),
                        [lxb_, con_b, xcvb], [xcvb])
                dve(lambda e, lx_=lx_, n=n: e.tensor_copy(out=halo[:, n, :], in_=lx_[:, TT:TT + 3]), [lxb_], [halo_b[n]])
                i2 = n % 2
                act(lambda e, xcv=xcv, i2=i2: e.activation(out=sqb[i2][:], in_=xcv[:], func=AF.Copy), [xcvb], [sq_b[i2]])
                pR, pRb = nps(); pI, pIb = nps()
                pe(lambda e, pR=pR, n=n, i2=i2: e.matmul(pR[:, 0:TT], lhsT=gwa[:, n, :], rhs=sqb[i2][:], start=True, stop=True), [gw_b, sq_b[i2]], [pRb])
                pe(lambda e, pI=pI, n=n, i2=i2: e.matmul(pI[:, 0:TT], lhsT=gwx[:, n, :], rhs=sqb[i2][:], start=True, stop=True), [gw_b, sq_b[i2]], [pIb])
                ta, tab = ntmp(); ti, tib = ntmp()
                act(lambda e, pR=pR, ta=ta, n=n, l=l: e.activation(out=ta[:], in_=pR[:, 0:TT], func=AF.Sigmoid, bias=lpar["gab"][:, l, n:n + 1], scale=1.0), [pRb, lpar_b], [tab])
                act(lambda e, pI=pI, ti=ti, n=n, l=l: e.activation(out=ti[:], in_=pI[:, 0:TT], func=AF.Sigmoid, bias=lpar["gxb"][:, l, n:n + 1], scale=1.0), [pIb, lpar_b], [tib])
                dve(lambda e, ti=ti, xcv=xcv: e.tensor_tensor(out=ti[:], in0=ti[:], in1=xcv[:], op=ALU.mult), [tib, xcvb], [tib])
                act(lambda e, ta=ta, tu=tu, n=n, l=l: e.activation(out=tu[:], in_=ta[:], func=AF.Exp, scale=cneg2[:, l, n:n + 1]), [tab, lpar_b], [tub])
                act(lambda e, ta=ta, n=n, l=l: e.activation(out=ta[:], in_=ta[:], func=AF.Exp, scale=cneg[:, l, n:n + 1]), [tab, lpar_b], [tab])
                dve(lambda e, tu=tu: e.tensor_scalar(out=tu[:], in0=tu[:], scalar1=-1.0, scalar2=1.0, op0=ALU.mult, op1=ALU.add), [tub], [tub])
                dve(lambda e, tu=tu: e.tensor_scalar(out=tu[:], in0=tu[:], scalar1=0.0, scalar2=None, op0=ALU.max), [tub], [tub])
                act(lambda e, tu=tu: e.activation(out=tu[:], in_=tu[:], func=AF.Sqrt), [tub], [tub])
                dve(lambda e, tu=tu, ti=ti: e.tensor_tensor(out=tu[:], in0=tu[:], in1=ti[:], op=ALU.mult), [tub, tib], [tub])
                dve(lambda e, ta=ta, tu=tu, ti=ti, n=n: e.tensor_tensor_scan(out=ti[:], data0=ta[:], data1=tu[:], initial=hst[:, n:n + 1], op0=ALU.mult, op1=ALU.add),
                    [tab, tub, h_b[n]], [tib])
                dve(lambda e, ti=ti, n=n: e.tensor_copy(out=hst[:, n:n + 1], in_=ti[:, TT - 1:TT]), [tib], [h_b[n]])
                act(lambda e, ti=ti, i2=i2: e.activation(out=sqb[i2][:], in_=ti[:], func=AF.Square), [tib], [sq_b[i2]])
                pe(lambda e, i2=i2, n=n: e.matmul(ssq[:, 0:TT], lhsT=ones[:], rhs=sqb[i2][:], start=(n == 0), stop=(n == NBL - 1)), [sq_b[i2], con_b], [ssqb])
                dve(lambda e, ti=ti, tg=tg, n=n, l=l: e.scalar_tensor_tensor(out=yT[:, 2 * H + n, :], in0=ti[:], scalar=lpar["lng"][:, l, n:n + 1], in1=tg[:], op0=ALU.mult, op1=ALU.mult),
                    [tib, tgb, lpar_b], [y_b])
            act(lambda e: e.activation(out=rstd[:], in_=ssq[:, 0:TT], func=AF.Sqrt, bias=float(EPS), scale=float(1.0 / LW)), [ssqb], [rstd_b])
            dve(lambda e: e.reciprocal(out=rstd[:], in_=rstd[:]), [rstd_b], [rstd_b])
            for n in range(NBL):
                dve(lambda e, n=n: e.tensor_tensor(out=yT[:, 2 * H + n, :], in0=yT[:, 2 * H + n, :], in1=rstd[:], op=ALU.mult), [rstd_b, y_b], [y_b])
            for g in range(DG):
                w_, wb_ = wload(f"wout{l}", g, MKC)
                for j in range(2):
                    p_, pb_ = nps()
                    for k in range(MKC):
                        pe(lambda e, p_=p_, w_=w_, j=j, k=k: e.matmul(p_[:, 0:TT], lhsT=w_[:, k, j * 128:(j + 1) * 128], rhs=yT[:, k, :], start=(k == 0), stop=(k == MKC - 1)),
                           [y_b, wb_], [pb_])
                    resid_update(t, 2 * g + j, p_, pb_, 2 * KD)
            norm_to_hn(t, 4 * KD, 3 * KD)
            if not moe:
                for half in range(2):
                    for gg in range(FH // 2):
                        g = half * (FH // 2) + gg
                        wg_, wgb = wload(f"wg{l}", g, KD)
                        wu_, wub = wload(f"wu{l}", g, KD)
                        for j in range(2):
                            pg, pgb = nps(); pu, pub = nps()
                            for k in range(KD):
                                pe(lambda e, pg=pg, wg_=wg_, j=j, k=k: e.matmul(pg[:, 0:TT], lhsT=wg_[:, k, j * 128:(j + 1) * 128], rhs=hnT[:, k, :], start=(k == 0), stop=(k == KD - 1)), [hn_b, wgb], [pgb])
                            for k in range(KD):
                                pe(lambda e, pu=pu, wu_=wu_, j=j, k=k: e.matmul(pu[:, 0:TT], lhsT=wu_[:, k, j * 128:(j + 1) * 128], rhs=hnT[:, k, :], start=(k == 0), stop=(k == KD - 1)), [hn_b, wub], [pub])
                            t_, tb = ntmp()
                            act(lambda e, pg=pg, t_=t_: e.activation(out=t_[:], in_=pg[:, 0:TT], func=AF.Silu), [pgb], [tb])
                            dve(lambda e, pu=pu, t_=t_, c=2 * gg + j: e.tensor_tensor(out=yT[:, c, :], in0=t_[:], in1=pu[:, 0:TT], op=ALU.mult), [pub, tb], [y_b])
                    for g in range(DG):
                        w_, wb_ = wload(f"wd{l}", half * DG + g, FH)
                        for j in range(2):
                            p_, pb_ = nps()
                            for k in range(FH):
                                pe(lambda e, p_=p_, w_=w_, j=j, k=k: e.matmul(p_[:, 0:TT], lhsT=w_[:, k, j * 128:(j + 1) * 128], rhs=yT[:, k, :], start=(k == 0), stop=(k == FH - 1)), [y_b, wb_], [pb_])
                            resid_update(t, 2 * g + j, p_, pb_, 5 * KD)
            else:
                for m in range(MS):
                    msl = slice(t0 + m * 128, t0 + (m + 1) * 128)
                    pl, plb = nps(6, 8)
                    for k in range(KD):
                        x_, xb = nxc()
                        load(x_[:, 0:128], xs[k, :, msl], xb, xs_b)
                        t_, tb = ntmp()
                        dve(lambda e, x_=x_, t_=t_, k=k, m=m: e.scalar_tensor_tensor(out=t_[:, 0:128], in0=x_[:, 0:128], scalar=mod[:, 4 * KD + k:4 * KD + k + 1], in1=rstd[:, m * 128:(m + 1) * 128],
                                                                                      op0=ALU.mult, op1=ALU.mult), [xb, rstd_b, mod_b], [tb])
                        dve(lambda e, t_=t_, k=k: e.tensor_scalar(out=t_[:, 0:128], in0=t_[:, 0:128], scalar1=mod[:, 3 * KD + k:3 * KD + k + 1], scalar2=None, op0=ALU.add), [tb, mod_b], [tb])
                        pe(lambda e, pl=pl, t_=t_, k=k: e.matmul(pl[:, 0:E], lhsT=t_[:, 0:128], rhs=rw[:, k, :], start=(k == 0), stop=(k == KD - 1)), [tb, rw_b], [plb])
                    lg = small[:, 16:16 + E]; m1 = small[:, 32:33]; m2 = small[:, 33:34]; eq1 = small[:, 40:40 + E]; w1 = small[:, 34:35]; w2 = small[:, 35:36]
                    dve(lambda e, pl=pl: e.tensor_copy(out=small[:, 16:16 + E], in_=pl[:, 0:E]), [plb], [small_b])
                    dve(lambda e: e.tensor_reduce(out=small[:, 32:33], in_=small[:, 16:16 + E], axis=mybir.AxisListType.X, op=ALU.max), [small_b], [small_b])
                    dve(lambda e: e.tensor_scalar(out=small[:, 40:40 + E], in0=small[:, 16:16 + E], scalar1=small[:, 32:33], scalar2=None, op0=ALU.is_ge), [small_b], [small_b])
                    dve(lambda e: e.scalar_tensor_tensor(out=small[:, 48:48 + E], in0=small[:, 40:40 + E], scalar=-1e30, in1=small[:, 16:16 + E], op0=ALU.mult, op1=ALU.add), [small_b], [small_b])
                    dve(lambda e: e.tensor_reduce(out=small[:, 33:34], in_=small[:, 48:48 + E], axis=mybir.AxisListType.X, op=ALU.max), [small_b], [small_b])
                    dve(lambda e: e.tensor_scalar(out=small[:, 56:56 + E], in0=small[:, 48:48 + E], scalar1=small[:, 33:34], scalar2=None, op0=ALU.is_ge), [small_b], [small_b])
                    dve(lambda e: e.tensor_tensor(out=small[:, 34:35], in0=small[:, 32:33], in1=small[:, 33:34], op=ALU.subtract), [small_b], [small_b])
                    act(lambda e: e.activation(out=small[:, 34:35], in_=small[:, 34:35], func=AF.Sigmoid), [small_b], [small_b])
                    dve(lambda e: e.tensor_scalar(out=small[:, 35:36], in0=small[:, 34:35], scalar1=-1.0, scalar2=1.0, op0=ALU.mult, op1=ALU.add), [small_b], [small_b])
                    dve(lambda e, m=m: e.tensor_scalar(out=comb[:, m, :], in0=small[:, 40:40 + E], scalar1=small[:, 34:35], scalar2=None, op0=ALU.mult), [small_b], [comb_b])
                    dve(lambda e, m=m: e.scalar_tensor_tensor(out=comb[:, m, :], in0=small[:, 56:56 + E], scalar=small[:, 35:36], in1=comb[:, m, :], op0=ALU.mult, op1=ALU.add), [small_b, comb_b], [comb_b])
                    pc, pcb = nps(6, 8)
                    pe(lambda e, pc=pc, m=m: e.transpose(out=pc[0:E, 0:128], in_=comb[:, m, :], identity=identf[:]), [comb_b, con_b], [pcb])
                    dve(lambda e, pc=pc, m=m: e.tensor_copy(out=combT[:, m * 128:(m + 1) * 128], in_=pc[0:E, 0:128]), [pcb], [combT_b])
                NG2 = DE // GW
                for qtr in range(NQ):
                    for el in range(EQ):
                        ex = qtr * EQ + el
                        pb2, pb2b = nps(6, 8)
                        pe(lambda e, pb2=pb2, ex=ex: e.matmul(pb2[:, 0:TT], lhsT=sel[:, ex, :], rhs=combT[:, :], start=True, stop=True), [sel_b, combT_b], [pb2b])
                        dve(lambda e, pb2=pb2: e.tensor_copy(out=combB[:], in_=pb2[:, 0:TT]), [pb2b], [combB_b])
                        for gg in range(NG2):
                            wg_, wgb = wload(f"wg{l}", ex * NG2 + gg, KD)
                            wu_, wub = wload(f"wu{l}", ex * NG2 + gg, KD)
                            for j in range(2):
                                pg, pgb = nps(); pu, pub = nps()
                                for k in range(KD):
                                    pe(lambda e, pg=pg, wg_=wg_, j=j, k=k: e.matmul(pg[:, 0:TT], lhsT=wg_[:, k, j * 128:(j + 1) * 128], rhs=hnT[:, k, :], start=(k == 0), stop=(k == KD - 1)), [hn_b, wgb], [pgb])
                                for k in range(KD):
                                    pe(lambda e, pu=pu, wu_=wu_, j=j, k=k: e.matmul(pu[:, 0:TT], lhsT=wu_[:, k, j * 128:(j + 1) * 128], rhs=hnT[:, k, :], start=(k == 0), stop=(k == KD - 1)), [hn_b, wub], [pub])
                                t_, tb = ntmp()
                                act(lambda e, pg=pg, t_=t_: e.activation(out=t_[:], in_=pg[:, 0:TT], func=AF.Silu), [pgb], [tb])
                                dve(lambda e, t_=t_, ex=ex: e.tensor_tensor(out=t_[:], in0=t_[:], in1=combB[:], op=ALU.mult), [tb, combB_b], [tb])
                                dve(lambda e, pu=pu, t_=t_, c=el * DEC + 2 * gg + j: e.tensor_tensor(out=yT[:, c, :], in0=t_[:], in1=pu[:, 0:TT], op=ALU.mult), [pub, tb], [y_b])
                    for g in range(DG):
                        w_, wb_ = wload(f"wd{l}", qtr * DG + g, QC)
                        for j in range(2):
                            p_, pb_ = nps()
                            for k in range(QC):
                                pe(lambda e, p_=p_, w_=w_, j=j, k=k: e.matmul(p_[:, 0:TT], lhsT=w_[:, k, j * 128:(j + 1) * 128], rhs=yT[:, k, :], start=(k == 0), stop=(k == QC - 1)), [y_b, wb_], [pb_])
                            resid_update(t, 2 * g + j, p_, pb_, 5 * KD)

    out_b = Buf("out")
    for t in range(NT):
        t0 = t * TT
        pst_, pstb = nps(6, 8)
        for k in range(KD):
            x_, xb = nxc()
            load(x_[:], xs[k, :, t0:t0 + TT], xb, xs_b)
            i = k % 2
            act(lambda e, x_=x_, i=i: e.activation(out=sqb[i][:], in_=x_[:], func=AF.Square), [xb], [sq_b[i]])
            pe(lambda e, i=i, k=k, pst_=pst_: e.matmul(pst_[:, 0:TT], lhsT=ones[:], rhs=sqb[i][:], start=(k == 0), stop=(k == KD - 1)), [sq_b[i], con_b], [pstb])
        act(lambda e, pst_=pst_: e.activation(out=rstd[:], in_=pst_[:, 0:TT], func=AF.Sqrt, bias=float(EPS), scale=float(1.0 / D)), [pstb], [rstd_b])
        dve(lambda e: e.reciprocal(out=rstd[:], in_=rstd[:]), [rstd_b], [rstd_b])
        for k in range(KD):
            x_, xb = nxc()
            load(x_[:], xs[k, :, t0:t0 + TT], xb, xs_b)
            dve(lambda e, x_=x_, k=k: e.scalar_tensor_tensor(out=x_[:], in0=x_[:], scalar=fngs[:, k:k + 1], in1=rstd[:], op0=ALU.mult, op1=ALU.mult), [xb, rstd_b, con_b], [xb])
            S.dma("pool", lambda e, sem, x_=x_, k=k, t0=t0: e.dma_start(out=outT[k, :, t0:t0 + TT], in_=x_[:]).then_inc(sem, 16), xb, out_b)

    S.emit(nc, st)
    st.close()
    return nc


def _tile_w(w, gw=GW):
    K, N = w.shape
    return np.ascontiguousarray(w.reshape(K // 128, 128, N // gw, gw).transpose(2, 1, 0, 3))


def _pp(v, nb):
    v = np.asarray(v)
    lead = v.shape[:-1]
    return np.ascontiguousarray(np.moveaxis(v.reshape(*lead, nb, 128), -1, 0))


def make_inputs(cfg, inp):
    D, H, LW, FF, E, DE, L, SEQ, TT, B = (cfg[k] for k in ("D", "H", "LW", "FF", "E", "DE", "L", "SEQ", "TT", "B"))
    KD = D // 128; Vw = H * 256; NBL = LW // 128; QK = H * 128; NCH = SEQ // 128
    f32 = np.float32
    shared = {}
    aw = np.asarray(inp["ada_w"], f32)
    shared["ada_w"] = np.ascontiguousarray(aw.reshape(KD, 128, 6 * KD, 128).transpose(2, 1, 0, 3))
    shared["ada_b"] = _pp(np.asarray(inp["ada_b"], f32), 6 * KD)
    shared["ada_tab"] = _pp(np.asarray(inp["ada_table"], f32).reshape(L, 6 * D), 6 * KD)
    shared["convw"] = _pp(np.asarray(inp["conv_w"], f32), NBL)
    for n, k in (("convb", "conv_b"), ("gab", "gate_a_b"), ("gxb", "gate_x_b"), ("lam", "lru_lambda"), ("lng", "lru_norm_g")):
        shared[n] = _pp(np.asarray(inp[k], f32), NBL)
    shared["rng"] = _pp(np.asarray(inp["ret_norm_g"], f32), 2 * H)
    shared["fng"] = _pp(np.asarray(inp["final_norm_g"], f32), KD)
    shared["gaw"] = np.ascontiguousarray(np.asarray(inp["gate_a_w"], f32).transpose(2, 0, 1, 3))
    shared["gxw"] = np.ascontiguousarray(np.asarray(inp["gate_x_w"], f32).transpose(2, 0, 1, 3))
    rw = np.asarray(inp["router_w"], f32)
    shared["router"] = np.ascontiguousarray(rw.reshape(rw.shape[0], KD, 128, E).transpose(2, 0, 1, 3))
    lg = np.log1p(-np.exp2(-5.0 - np.arange(H, dtype=np.float64)))
    idx = np.arange(128, dtype=np.float64)
    rel = idx[None, :] - idx[:, None]
    maskT = np.where(rel[None] >= 0, np.exp(lg[:, None, None] * np.maximum(rel[None], 0)), 0.0) * (128 ** -0.5)
    shared["maskT"] = np.ascontiguousarray(maskT.transpose(1, 0, 2)).astype(f32)
    shared["qdec"] = np.ascontiguousarray(np.broadcast_to(np.exp(lg[:, None] * (idx[None, :] + 1.0))[None], (128, H, 128))).astype(f32)
    shared["kdec"] = np.ascontiguousarray((np.exp(lg[None, :] * (127.0 - idx[:, None])) * (128 ** -0.5))).astype(f32)
    shared["gC"] = np.ascontiguousarray(np.broadcast_to(np.exp(lg * 128.0)[None], (128, H))).astype(f32)
    invf = np.exp2(-np.arange(0, 128, 2, dtype=f32) / f32(128) * np.log2(f32(10000.0))).astype(f32)
    shared["invf"] = np.ascontiguousarray(np.broadcast_to(invf[None], (128, 64)))
    shared["ident"] = np.eye(128, dtype=f32)
    S0, S1, S2, S3, S4 = QK, 2 * QK, 2 * QK + Vw, 2 * QK + 2 * Vw, 2 * QK + 2 * Vw + LW
    for l in range(L):
        w = np.asarray(inp["w_in"][l], f32)
        cols = []
        for h in range(H):
            cols += [w[:, h * 128:(h + 1) * 128], w[:, S0 + h * 128:S0 + (h + 1) * 128], w[:, S1 + h * 256:S1 + (h + 1) * 256], w[:, S2 + h * 256:S2 + (h + 1) * 256]]
        for n in range(NBL):
            cols += [w[:, S3 + n * 128:S3 + (n + 1) * 128], w[:, S4 + n * 128:S4 + (n + 1) * 128]]
        shared[f"win{l}"] = _tile_w(np.concatenate(cols, axis=1))
        shared[f"wout{l}"] = _tile_w(np.asarray(inp["w_out"][l], f32))
        if l % 2 == 0:
            i = l // 2
            shared[f"wg{l}"] = _tile_w(np.asarray(inp["ffn_w_gate"][i], f32)); shared[f"wu{l}"] = _tile_w(np.asarray(inp["ffn_w_up"][i], f32))
            wd = np.asarray(inp["ffn_w_down"][i], f32)
            shared[f"wd{l}"] = np.concatenate([_tile_w(wd[hh * FF // 2:(hh + 1) * FF // 2]) for hh in range(2)], axis=0)
        else:
            i = l // 2
            mg = np.asarray(inp["moe_w_gate"][i], f32); mu = np.asarray(inp["moe_w_up"][i], f32); md = np.asarray(inp["moe_w_down"][i], f32)
            shared[f"wg{l}"] = np.concatenate([_tile_w(mg[e]) for e in range(E)], axis=0)
            shared[f"wu{l}"] = np.concatenate([_tile_w(mu[e]) for e in range(E)], axis=0)
            mdf = md.reshape(E * DE, D)
            q = E * DE // 4
            shared[f"wd{l}"] = np.concatenate([_tile_w(mdf[qq * q:(qq + 1) * q]) for qq in range(4)], axis=0)
    maps = []
    x = np.asarray(inp["x"], f32); c = np.asarray(inp["c"], f32); pos = np.asarray(inp["positions"], np.int32)
    for b in range(B):
        m = dict(shared)
        m["xT"] = np.ascontiguousarray(x[b].T.reshape(KD, 128, SEQ))
        m["cT"] = _pp(c[b], KD)
        m["pos"] = np.ascontiguousarray(pos[b].reshape(NCH, 128).T)
        maps.append(m)
    return maps


def run(cfg, inp):
    nc = build(cfg)
    maps = make_inputs(cfg, inp)
    B = cfg["B"]
    res = run_bass_kernel_spmd(nc, maps, core_ids=list(range(B)))
    D, SEQ = cfg["D"], cfg["SEQ"]
    out = np.stack([np.asarray(res.results[b]["outT"]).reshape(D, SEQ).T for b in range(B)], axis=0)
    return np.ascontiguousarray(out.astype(np.float32))


def kernel(**inputs):
    return run(FULL, inputs)
```

```python
import contextlib
import numpy as np
import concourse.bass as bass
import concourse.mybir as mybir
from concourse.bass_utils import run_bass_kernel_spmd

ALU = mybir.AluOpType
AF = mybir.ActivationFunctionType
F32, BF16, I32 = mybir.dt.float32, mybir.dt.bfloat16, mybir.dt.int32
GW = 256
EPS = 1e-6
SAME_ENG_SYNC = True
STORE_ENG = "pool"

FULL = dict(D=4096, H=8, LW=2048, FF=8192, E=8, DE=1792, L=4, SEQ=8192, TT=512, B=2)


class Buf:
    __slots__ = ("name", "w", "rs", "cnt", "sem")

    def __init__(self, name):
        self.name = name; self.w = None; self.rs = []; self.cnt = 0; self.sem = None


class Sched:
    ENGS = ("pe", "act", "dve", "pool", "sp")

    def __init__(self):
        self.q = {e: [] for e in self.ENGS}
        self.dmabufs = []

    def _deps(self, rd, wr):
        deps = []
        for b in rd:
            if b.w is not None:
                deps.append(b.w)
        for b in wr:
            if b.w is not None:
                deps.append(b.w)
            deps.extend(b.rs)
        return deps

    def op(self, eng, fn, rd=(), wr=()):
        deps = self._deps(rd, wr)
        ev = ("e", eng, len(self.q[eng]))
        self.q[eng].append([fn, deps, False, None])
        for b in rd:
            b.rs.append(ev)
        for b in wr:
            b.w = ev; b.rs = []
        return ev

    def dma(self, eng, fn, src, dst, n=1, slot=None):
        deps = self._deps([src], [dst])
        if slot is None:
            slot = dst
        elif slot.w is not None:
            deps.append(slot.w)
        if slot.sem is None:
            slot.sem = True; self.dmabufs.append(slot)
        slot.cnt += 16 * n
        ev = ("d", slot, slot.cnt)
        if slot is not dst:
            slot.w = ev
        self.q[eng].append([fn, deps, False, slot])
        src.rs.append(ev)
        dst.w = ev; dst.rs = []
        return ev

    def emit(self, nc, stack):
        for e in self.ENGS:
            for ins in self.q[e]:
                for d in ins[1]:
                    if d[0] == "e":
                        if d[1] == "pe" and e == "pe":
                            continue
                        if d[1] == e and not SAME_ENG_SYNC:
                            continue
                        self.q[d[1]][d[2]][2] = True
        cum = {}
        for e in self.ENGS:
            c = 0; arr = []
            for ins in self.q[e]:
                if ins[2]:
                    c += 1
                arr.append(c)
            cum[e] = arr
        esem = {e: stack.enter_context(nc.semaphore("es_" + e)) for e in self.ENGS}
        for b in self.dmabufs:
            b.sem = stack.enter_context(nc.semaphore("ds_" + b.name))
        engobj = {"pe": "tensor", "act": "scalar", "dve": "vector", "pool": "gpsimd", "sp": "sync"}
        block = stack.enter_context(nc.Block())
        finals = [(b.sem, b.cnt) for b in self.dmabufs]

        def run(e):
            def body(eng):
                waited = {}
                for idx, ins in enumerate(self.q[e]):
                    need = {}
                    for d in ins[1]:
                        if d[0] == "e":
                            if d[1] == "pe" and e == "pe":
                                continue
                            if d[1] == e and not SAME_ENG_SYNC:
                                continue
                            key = ("e", d[1]); sem = esem[d[1]]; val = cum[d[1]][d[2]]
                        else:
                            key = ("d", id(d[1])); sem = d[1].sem; val = d[2]
                        if val > need.get(key, (None, 0))[1]:
                            need[key] = (sem, val)
                    for key, (sem, val) in need.items():
                        if waited.get(key, 0) >= val:
                            continue
                        waited[key] = val
                        eng.wait_ge(sem, val)
                    if ins[3] is not None:
                        ins[0](eng, ins[3].sem)
                    else:
                        r = ins[0](eng)
                        if ins[2]:
                            r.then_inc(esem[e], 1)
                if e == "sp":
                    for sem, cnt in finals:
                        eng.wait_ge(sem, cnt)
            return body
        for e in self.ENGS:
            getattr(block, engobj[e])(run(e))


def build(cfg):
    D, H, LW, FF, E, DE, L, SEQ, TT = (cfg[k] for k in ("D", "H", "LW", "FF", "E", "DE", "L", "SEQ", "TT"))
    KD = D // 128; Vw = H * 256; NBL = LW // 128; MIX = Vw + LW; MKC = MIX // 128
    NT = SEQ // TT; MS = TT // 128; NCH = SEQ // 128
    NBI = (2 * H * 128 + 2 * Vw + 2 * LW) // GW
    DG = D // GW
    FH = FF // 128 // 2
    NQ = 4; EQ = E // NQ; DEC = DE // 128; QC = EQ * DEC
    nc = bass.Bass("TRN2", target_bir_lowering=False)
    S = Sched()
    st = contextlib.ExitStack()

    def din(name, shape, dt=F32):
        return nc.dram_tensor(name, list(shape), dt, kind="ExternalInput").ap()

    def dint(name, shape, dt):
        return nc.dram_tensor(name, list(shape), dt, kind="Internal").ap()

    xT_in = din("xT", [KD, 128, SEQ])
    outT = nc.dram_tensor("outT", [KD, 128, SEQ], F32, kind="ExternalOutput").ap()
    xs = dint("xs", [KD, 128, SEQ], F32)
    cT = din("cT", [128, KD]); pos = din("pos", [128, NCH], I32)
    ada_w = din("ada_w", [6 * KD, 128, KD, 128]); ada_b = din("ada_b", [128, 6 * KD]); ada_tab = din("ada_tab", [128, L, 6 * KD])
    convw = din("convw", [128, L, 4, NBL])
    lp = {n: din(n, [128, L, NBL]) for n in ("convb", "gab", "gxb", "lam", "lng")}
    rng = din("rng", [128, L, 2 * H]); fng = din("fng", [128, KD])
    gaw = din("gaw", [128, L, NBL, 128]); gxw = din("gxw", [128, L, NBL, 128])
    router = din("router", [128, max(L // 2, 1), KD, E])
    maskT_d = din("maskT", [128, H, 128]); qdec_d = din("qdec", [128, H, 128]); kdec_d = din("kdec", [128, H]); gC_d = din("gC", [128, H])
    invf_d = din("invf", [128, 64]); ident_d = din("ident", [128, 128])

    wspec = {}
    for l in range(L):
        wspec[f"win{l}"] = [NBI, 128, KD, GW]
        wspec[f"wout{l}"] = [DG, 128, MKC, GW]
        if l % 2 == 0:
            wspec[f"wg{l}"] = [FF // GW, 128, KD, GW]; wspec[f"wu{l}"] = [FF // GW, 128, KD, GW]
            wspec[f"wd{l}"] = [2 * DG, 128, FH, GW]
        else:
            wspec[f"wg{l}"] = [E * DE // GW, 128, KD, GW]; wspec[f"wu{l}"] = [E * DE // GW, 128, KD, GW]
            wspec[f"wd{l}"] = [NQ * DG, 128, QC, GW]
    wf, wb, wbuf = {}, {}, {}
    for n, shp in wspec.items():
        wf[n] = din(n, shp); wb[n] = dint(n + "_b", shp, BF16); wbuf[n] = Buf(n)
    ext = Buf("ext")

    def sb(name, shape, dt=F32):
        return st.enter_context(nc.sbuf_tensor("sb_" + name, list(shape), dt))

    def ps(name):
        return st.enter_context(nc.psum_tensor(name, [128, 512], F32))

    NWT = 3
    wt = [sb(f"wt{i}", [128, 32, GW], BF16) for i in range(NWT)]; wt_b = [Buf(f"wt{i}") for i in range(NWT)]
    hnT = sb("hnT", [128, KD, TT], BF16); hn_b = Buf("hnT")
    yT = sb("yT", [128, max(MKC, FH, QC), TT], BF16); y_b = Buf("yT")
    NXC = 3
    xc = [sb(f"xc{i}", [128, TT]) for i in range(NXC)]; xc_b = [Buf(f"xc{i}") for i in range(NXC)]
    NTMP = 7
    tmp = [sb(f"tmp{i}", [128, TT]) for i in range(NTMP)]; tmp_b = [Buf(f"tmp{i}") for i in range(NTMP)]
    sqb = [sb(f"sq{i}", [128, TT], BF16) for i in range(2)]; sq_b = [Buf(f"sq{i}") for i in range(2)]
    rstd = sb("rstd", [128, TT]); rstd_b = Buf("rstd")
    cosT = sb("cosT", [128, MS, 64]); sinT = sb("sinT", [128, MS, 64]); rope_b = Buf("rope")
    mod = sb("mod", [128, 6 * KD]); modb = sb("modb", [128, 6 * KD]); mod_b = Buf("mod"); modb_b = Buf("modb")
    tabs = sb("tabs", [128, 6 * KD]); tabs_b = Buf("tabs")
    ident = sb("ident", [128, 128], BF16); identf = sb("identf", [128, 128]); ones = sb("ones", [128, 128], BF16); con_b = Buf("consts")
    maskT = sb("maskT", [128, H, 128]); qdec = sb("qdec", [128, H, 128]); kdec = sb("kdec", [128, H]); gC = sb("gC", [128, H])
    Sst = sb("Sst", [128, H, 256]); Sbf = sb("Sbf", [128, H, 256], BF16); S_b = [Buf(f"S{h}") for h in range(H)]
    hst = sb("hst", [128, NBL]); h_b = [Buf(f"h{n}") for n in range(NBL)]
    halo = sb("halo", [128, NBL, 3]); halo_b = [Buf(f"halo{n}") for n in range(NBL)]
    lxt = [sb(f"lxt{i}", [128, 3 + TT]) for i in range(2)]; lxt_b = [Buf(f"lxt{i}") for i in range(2)]
    lpar = {n: sb("s_" + n, [128, L, NBL]) for n in lp}; cw = sb("cw", [128, L, 4, NBL]); cneg = sb("cneg", [128, L, NBL]); cneg2 = sb("cneg2", [128, L, NBL]); lpar_b = Buf("lpar")
    gwa = sb("gwa", [128, NBL, 128], BF16); gwx = sb("gwx", [128, NBL, 128], BF16); gw_b = Buf("gw")
    rngs = sb("rngs", [128, L, 2 * H]); rng_b = con_b
    fngs = sb("fngs", [128, KD])
    rw = sb("rw", [128, KD, E]); rw_b = Buf("rw")
    qks = sb("qks", [128, 2, 128], BF16); qks_b = Buf("qks")
    kds = sb("kds", [128, MS, 128], BF16); kds_b = Buf("kds")
    qTs = sb("qTs", [128, 3, TT], BF16); qT_b = Buf("qTs")
    vs = sb("vs", [128, MS, 256], BF16); vs_b = Buf("vs")
    gs = sb("gs", [128, MS, 256], BF16); gs_b = Buf("gs")
    sTs = [sb(f"sT{i}", [128, 128], BF16) for i in range(2)]; sT_b = [Buf(f"sT{i}") for i in range(2)]
    yrs = [sb(f"yr{i}", [128, 256]) for i in range(2)]; yr_b = [Buf(f"yr{i}") for i in range(2)]
    yrb = [sb(f"yrb{i}", [128, 256], BF16) for i in range(2)]; yrb_b = [Buf(f"yrb{i}") for i in range(2)]
    bst = sb("bst", [128, 8]); bst_b = Buf("bst")
    comb = sb("comb", [128, MS, E]); comb_b = Buf("comb"); combT = sb("combT", [E, TT]); combT_b = Buf("combT")
    sel = sb("sel", [E, E, 128]); combB = sb("combB", [128, TT]); combB_b = Buf("combB"); sel_b = Buf("sel")
    small = sb("small", [128, 64]); small_b = Buf("small")
    lgT = sb("lgT", [E, TT]); lgT_b = Buf("lgT"); cshift = sb("cshift", [E, 1]); cshift_b = Buf("cshift")
    cs = sb("cs", [128, KD]); cs_b = Buf("cs")
    pst = [ps(f"ps{i}") for i in range(8)]; ps_b = [Buf(f"ps{i}") for i in range(8)]
    psrr = [0]

    def nps(lo=0, hi=6):
        i = lo + psrr[0] % (hi - lo); psrr[0] += 1
        return pst[i], ps_b[i]
    trr = [0]

    def ntmp():
        i = trr[0] % NTMP; trr[0] += 1
        return tmp[i], tmp_b[i]
    xrr = [0]

    def nxc():
        i = xrr[0] % NXC; xrr[0] += 1
        return xc[i], xc_b[i]

    def load(dst_ap, src_ap, dbuf, sbuf=ext, eng="sp"):
        S.dma(eng, lambda e, sem: e.dma_start(out=dst_ap, in_=src_ap).then_inc(sem, 16), sbuf, dbuf)

    def dve(fn, rd, wr):
        S.op("dve", fn, rd, wr)

    def act(fn, rd, wr):
        S.op("act", fn, rd, wr)

    def pe(fn, rd, wr):
        S.op("pe", fn, rd, wr)

    def cast_weight(n):
        tot = int(np.prod(wspec[n])); rows = tot // 2048
        src = wf[n].rearrange("a p k c -> (a p k c)").rearrange("(r c) -> r c", c=2048)
        dst = wb[n].rearrange("a p k c -> (a p k c)").rearrange("(r c) -> r c", c=2048)
        step = 4096
        for r0 in range(0, rows, step):
            r1 = min(rows, r0 + step)
            S.dma("pool", lambda e, sem, r0=r0, r1=r1: e.dma_start(out=dst[r0:r1, :], in_=src[r0:r1, :]).then_inc(sem, 16), ext, wbuf[n])
    load(gwa[:], gaw[:, 0], gw_b, eng="pool")
    load(gwx[:], gxw[:, 0], gw_b, eng="pool")
    for l in range(L):
        for n in (f"win{l}", f"wout{l}", f"wg{l}", f"wu{l}", f"wd{l}"):
            cast_weight(n)

    load(identf[:], ident_d, con_b)
    dve(lambda e: e.tensor_copy(out=ident[:], in_=identf[:]), [con_b], [con_b])
    dve(lambda e: e.memset(ones[:], 1.0), [], [con_b])
    for t_, d_ in ((maskT, maskT_d), (qdec, qdec_d), (kdec, kdec_d), (gC, gC_d), (cw, convw), (fngs, fng), (rngs, rng)):
        load(t_[:], d_, con_b)
    for n in lp:
        load(lpar[n][:], lp[n], lpar_b)
    act(lambda e: e.activation(out=cneg[:], in_=lpar["lam"][:], func=AF.Exp, scale=-1.0), [lpar_b], [lpar_b])
    act(lambda e: e.activation(out=cneg[:], in_=cneg[:], func=AF.Ln, bias=1.0, scale=1.0), [lpar_b], [lpar_b])
    dve(lambda e: e.tensor_scalar(out=cneg2[:], in0=cneg[:], scalar1=-16.0, scalar2=None, op0=ALU.mult), [lpar_b], [lpar_b])
    dve(lambda e: e.tensor_scalar(out=cneg[:], in0=cneg[:], scalar1=-8.0, scalar2=None, op0=ALU.mult), [lpar_b], [lpar_b])
    dve(lambda e: e.memset(Sst[:], 0.0), [], S_b)
    dve(lambda e: e.memset(Sbf[:], 0.0), [], S_b)
    dve(lambda e: e.memset(hst[:], 0.0), [], h_b)
    dve(lambda e: e.memset(halo[:], 0.0), [], halo_b)
    dve(lambda e: e.memset(sel[:], 0.0), [], [sel_b])
    dve(lambda e: e.tensor_copy(out=sel[:, :, :], in_=identf[0:E, 0:E].unsqueeze(2).broadcast_to([E, E, 128])), [con_b, sel_b], [sel_b])

    posi = st.enter_context(nc.sbuf_tensor("sb_posi", [128, NCH], I32)); posf = sb("posf", [128, NCH]); invf = sb("invf", [128, 64])
    angb = sb("angb", [128, MS, 64]); angn = sb("angn", [128, MS, 64]); angi = st.enter_context(nc.sbuf_tensor("sb_angi", [128, MS, 64], I32))
    load(posi[:], pos, rope_b); load(invf[:], invf_d, rope_b)
    dve(lambda e: e.tensor_copy(out=posf[:], in_=posi[:]), [rope_b], [rope_b])
    TWO_PI = 2.0 * np.pi; C1 = 6.28125; C2 = TWO_PI - C1

    def rope_tile(t):
        for table, shift in ((sinT, 0.0), (cosT, 0.5 * np.pi)):
            dve(lambda e: e.tensor_tensor(out=angb[:], in0=posf[:, t * MS:(t + 1) * MS].unsqueeze(2).broadcast_to([128, MS, 64]),
                                          in1=invf[:].unsqueeze(1).broadcast_to([128, MS, 64]), op=ALU.mult), [rope_b], [rope_b])
            if shift:
                dve(lambda e, s_=shift: e.tensor_scalar(out=angb[:], in0=angb[:], scalar1=float(s_), scalar2=None, op0=ALU.add), [rope_b], [rope_b])
            dve(lambda e: e.tensor_scalar(out=angn[:], in0=angb[:], scalar1=float(1.0 / TWO_PI), scalar2=None, op0=ALU.mult), [rope_b], [rope_b])
            dve(lambda e: e.tensor_copy(out=angi[:], in_=angn[:]), [rope_b], [rope_b])
            dve(lambda e: e.tensor_copy(out=angn[:], in_=angi[:]), [rope_b], [rope_b])
            dve(lambda e: e.scalar_tensor_tensor(out=angb[:], in0=angn[:], scalar=float(-C1), in1=angb[:], op0=ALU.mult, op1=ALU.add), [rope_b], [rope_b])
            dve(lambda e: e.scalar_tensor_tensor(out=angb[:], in0=angn[:], scalar=float(-C2), in1=angb[:], op0=ALU.mult, op1=ALU.add), [rope_b], [rope_b])
            dve(lambda e: e.tensor_scalar(out=angn[:], in0=angb[:], scalar1=float(np.pi), scalar2=float(-TWO_PI), op0=ALU.is_gt, op1=ALU.mult), [rope_b], [rope_b])
            dve(lambda e: e.tensor_tensor(out=angb[:], in0=angb[:], in1=angn[:], op=ALU.add), [rope_b], [rope_b])
            dve(lambda e: e.tensor_scalar(out=angn[:], in0=angb[:], scalar1=float(-np.pi), scalar2=float(TWO_PI), op0=ALU.is_lt, op1=ALU.mult), [rope_b], [rope_b])
            dve(lambda e: e.tensor_tensor(out=angb[:], in0=angb[:], in1=angn[:], op=ALU.add), [rope_b], [rope_b])
            act(lambda e, t_=table: e.activation(out=t_[:], in_=angb[:], func=AF.Sin), [rope_b], [rope_b])

    load(cs[:], cT, cs_b)
    act(lambda e: e.activation(out=cs[:], in_=cs[:], func=AF.Silu), [cs_b], [cs_b])
    load(modb[:], ada_b, modb_b)
    yTf = yT[:, 0:KD, 0:256].bitcast(F32) if TT >= 256 else None
    adaw_t = [yTf[:, :, 0:128]]; adaw_b = [y_b]
    for g in range(6 * KD):
        i = 0
        load(adaw_t[i], ada_w[g], adaw_b[i])
        p_, pb_ = nps(6, 8)
        for k in range(KD):
            pe(lambda e, p_=p_, i=i, k=k: e.matmul(p_[:, 0:1], lhsT=adaw_t[i][:, k, :], rhs=cs[:, k:k + 1], start=(k == 0), stop=(k == KD - 1)),
               [adaw_b[i], cs_b], [pb_])
        dve(lambda e, p_=p_, g=g: e.tensor_tensor(out=modb[:, g:g + 1], in0=p_[:, 0:1], in1=modb[:, g:g + 1], op=ALU.add), [pb_, modb_b], [modb_b])

    xs_bt = [[Buf(f"xs{k}_{t}") for t in range(NT)] for k in range(KD)]
    xsrc = [[xT_in for t in range(NT)] for k in range(KD)]
    NSLOT = 8
    st_slots = [Buf(f"stslot{i}") for i in range(NSLOT)]
    st_rr = [0]

    def xstore(dst_ap, src_ap, sbuf, dbuf):
        sl = st_slots[st_rr[0] % NSLOT]; st_rr[0] += 1
        S.dma(STORE_ENG, lambda e, sem: e.dma_start(out=dst_ap, in_=src_ap).then_inc(sem, 16), sbuf, dbuf, slot=sl)

    wrr = [0]

    def wload(name, idx, kc):
        i = wrr[0] % NWT; wrr[0] += 1
        S.dma("sp", lambda e, sem, i=i: e.dma_start(out=wt[i][:, 0:kc, :], in_=wb[name][idx]).then_inc(sem, 16), wbuf[name], wt_b[i])
        return wt[i], wt_b[i]

    def norm_to_hn(t, a_off, s_off, stats=None, router=False):
        t0 = t * TT
        if stats is not None:
            pst_, pstb = stats
        else:
            pst_, pstb = nps(6, 8)
            for k in range(KD):
                x_, xb = nxc()
                load(x_[:], xsrc[k][t][k, :, t0:t0 + TT], xb, xs_bt[k][t])
                i = k % 2
                act(lambda e, x_=x_, i=i: e.activation(out=sqb[i][:], in_=x_[:], func=AF.Square), [xb], [sq_b[i]])
                pe(lambda e, i=i, k=k: e.matmul(pst_[:, 0:TT], lhsT=ones[:], rhs=sqb[i][:], start=(k == 0), stop=(k == KD - 1)), [sq_b[i], con_b], [pstb])
        act(lambda e: e.activation(out=rstd[:], in_=pst_[:, 0:TT], func=AF.Sqrt, bias=float(EPS), scale=float(1.0 / D)), [pstb], [rstd_b])
        dve(lambda e: e.reciprocal(out=rstd[:], in_=rstd[:]), [rstd_b], [rstd_b])
        for k in range(KD):
            x_, xb = nxc()
            load(x_[:], xsrc[k][t][k, :, t0:t0 + TT], xb, xs_bt[k][t])
            t_, tb = ntmp()
            dve(lambda e, x_=x_, t_=t_, k=k: e.scalar_tensor_tensor(out=t_[:], in0=x_[:], scalar=mod[:, a_off + k:a_off + k + 1], in1=rstd[:],
                                                                    op0=ALU.mult, op1=ALU.mult), [xb, rstd_b, mod_b], [tb])
            act(lambda e, t_=t_, k=k: e.activation(out=hnT[:, k, :], in_=t_[:], func=AF.Identity, bias=mod[:, s_off + k:s_off + k + 1], scale=1.0),
                [tb, mod_b], [hn_b])
            if router:
                pe(lambda e, t_=t_, k=k: e.matmul(pst[7][0:E, 0:TT], lhsT=rw[:, k, :], rhs=t_[:], start=(k == 0), stop=(k == KD - 1)), [tb, rw_b], [ps_b[7]])
        if router:
            dve(lambda e: e.tensor_scalar(out=lgT[:], in0=pst[7][0:E, 0:TT], scalar1=cshift[:, 0:1], scalar2=None, op0=ALU.add), [ps_b[7], cshift_b], [lgT_b])

    def resid_update(t, dch, p_, pb_, g_off, stats=None):
        t0 = t * TT
        x_, xb = nxc()
        load(x_[:], xsrc[dch][t][dch, :, t0:t0 + TT], xb, xs_bt[dch][t])
        dve(lambda e: e.scalar_tensor_tensor(out=x_[:], in0=p_[:, 0:TT], scalar=mod[:, g_off + dch:g_off + dch + 1], in1=x_[:], op0=ALU.mult, op1=ALU.add),
            [pb_, xb, mod_b], [xb])
        xstore(xs[dch, :, t0:t0 + TT], x_[:], xb, xs_bt[dch][t])
        xsrc[dch][t] = xs
        if stats is None:
            return None
        sp_, spb_ = stats
        i = dch % 2
        act(lambda e: e.activation(out=sqb[i][:], in_=x_[:], func=AF.Square), [xb], [sq_b[i]])

        def deferred():
            pe(lambda e: e.matmul(sp_[:, 0:TT], lhsT=ones[:], rhs=sqb[i][:], start=(dch == 0), stop=(dch == KD - 1)), [sq_b[i], con_b], [spb_])
        return deferred

    def gemm_fm(name, idx, kc, src, srcb, j, p_, pb_):
        pass

    for l in range(L):
        moe = (l % 2 == 1)
        load(tabs[:], ada_tab[:, l, :], tabs_b)
        dve(lambda e, l=l: e.tensor_tensor(out=mod[:], in0=modb[:], in1=tabs[:], op=ALU.add), [modb_b, tabs_b], [mod_b])
        for j in (1, 4):
            dve(lambda e, j=j: e.tensor_scalar(out=mod[:, j * KD:(j + 1) * KD], in0=mod[:, j * KD:(j + 1) * KD], scalar1=1.0, scalar2=None, op0=ALU.add), [mod_b], [mod_b])
        if l > 0:
            load(gwa[:], gaw[:, l], gw_b, eng="pool")
            load(gwx[:], gxw[:, l], gw_b, eng="pool")
        if moe:
            load(rw[:], router[:, l // 2], rw_b)
            pcs, pcsb = nps(6, 8)
            for k in range(KD):
                pe(lambda e, pcs=pcs, k=k: e.matmul(pcs[0:E, 0:1], lhsT=rw[:, k, :], rhs=mod[:, 3 * KD + k:3 * KD + k + 1], start=(k == 0), stop=(k == KD - 1)), [rw_b, mod_b], [pcsb])
            dve(lambda e, pcs=pcs: e.tensor_copy(out=cshift[:], in_=pcs[0:E, 0:1]), [pcsb], [cshift_b])
        dve(lambda e: e.memset(Sst[:], 0.0), [], S_b)
        dve(lambda e: e.memset(Sbf[:], 0.0), [], S_b)
        dve(lambda e: e.memset(hst[:], 0.0), [], h_b)
        dve(lambda e: e.memset(halo[:], 0.0), [], halo_b)

        for t in range(NT):
            t0 = t * TT
            rope_tile(t)
            norm_to_hn(t, 1 * KD, 0 * KD)
            for h in range(H):
                w_, wb_ = wload(f"win{l}", 3 * h, KD)
                for m in range(MS):
                    p_, pb_ = nps()
                    for k in range(KD):
                        pe(lambda e, p_=p_, w_=w_, m=m, k=k: e.matmul(p_[:, 0:GW], lhsT=hnT[:, k, m * 128:(m + 1) * 128], rhs=w_[:, k, :], start=(k == 0), stop=(k == KD - 1)),
                           [hn_b, wb_], [pb_])
                    ci = m
                    pv = p_[:, 0:GW].rearrange("p (a b c) -> p a b c", a=2, b=2)
                    t1, tb1 = ntmp(); t2, tb2 = ntmp()
                    cosb = cosT[:, ci, :].unsqueeze(1).broadcast_to([128, 2, 64]); sinb = sinT[:, ci, :].unsqueeze(1).broadcast_to([128, 2, 64])
                    a1 = t1[:, 0:128].rearrange("p (a c) -> p a c", a=2); a2 = t1[:, 128:256].rearrange("p (a c) -> p a c", a=2)
                    b1 = t2[:, 0:128].rearrange("p (a c) -> p a c", a=2); b2 = t2[:, 128:256].rearrange("p (a c) -> p a c", a=2)
                    qv = qks[:].rearrange("p a (b c) -> p a b c", b=2)
                    dve(lambda e, pv=pv, a1=a1, cosb=cosb: e.tensor_tensor(out=a1, in0=pv[:, :, 0, :], in1=cosb, op=ALU.mult), [pb_, rope_b], [tb1])
                    dve(lambda e, pv=pv, a2=a2, sinb=sinb: e.tensor_tensor(out=a2, in0=pv[:, :, 1, :], in1=sinb, op=ALU.mult), [pb_, rope_b], [tb1])
                    dve(lambda e, pv=pv, b1=b1, sinb=sinb: e.tensor_tensor(out=b1, in0=pv[:, :, 0, :], in1=sinb, op=ALU.mult), [pb_, rope_b], [tb2])
                    dve(lambda e, pv=pv, b2=b2, cosb=cosb: e.tensor_tensor(out=b2, in0=pv[:, :, 1, :], in1=cosb, op=ALU.mult), [pb_, rope_b], [tb2])
                    dve(lambda e, qv=qv, a1=a1, a2=a2: e.tensor_tensor(out=qv[:, :, 0, :], in0=a1, in1=a2, op=ALU.subtract), [tb1], [qks_b])
                    dve(lambda e, qv=qv, b1=b1, b2=b2: e.tensor_tensor(out=qv[:, :, 1, :], in0=b1, in1=b2, op=ALU.add), [tb2], [qks_b])
                    dve(lambda e, m=m, h=h: e.tensor_scalar(out=kds[:, m, :], in0=qks[:, 1, :], scalar1=kdec[:, h:h + 1], scalar2=None, op0=ALU.mult), [qks_b, con_b], [kds_b])
                    pt, ptb = nps(6, 8)
                    ptv = pt[:].bitcast(BF16)
                    pe(lambda e, ptv=ptv: e.transpose(out=ptv[:, 0:128], in_=qks[:, 0, :], identity=ident[:]), [qks_b, con_b], [ptb])
                    pe(lambda e, ptv=ptv: e.transpose(out=ptv[:, 128:256], in_=qks[:, 1, :], identity=ident[:]), [qks_b, con_b], [ptb])
                    act(lambda e, ptv=ptv, m=m: e.activation(out=qTs[:, 0, m * 128:(m + 1) * 128], in_=ptv[:, 0:128], func=AF.Copy), [ptb], [qT_b])
                    dve(lambda e, ptv=ptv, m=m, h=h: e.tensor_tensor(out=qTs[:, 1, m * 128:(m + 1) * 128], in0=ptv[:, 0:128], in1=qdec[:, h, :], op=ALU.mult), [ptb, con_b], [qT_b])
                    act(lambda e, ptv=ptv, m=m: e.activation(out=qTs[:, 2, m * 128:(m + 1) * 128], in_=ptv[:, 128:256], func=AF.Copy), [ptb], [qT_b])
                w_, wb_ = wload(f"win{l}", 3 * h + 1, KD)
                for m in range(MS):
                    p_, pb_ = nps()
                    for k in range(KD):
                        pe(lambda e, p_=p_, w_=w_, m=m, k=k: e.matmul(p_[:, 0:GW], lhsT=hnT[:, k, m * 128:(m + 1) * 128], rhs=w_[:, k, :], start=(k == 0), stop=(k == KD - 1)),
                           [hn_b, wb_], [pb_])
                    act(lambda e, p_=p_, m=m: e.activation(out=vs[:, m, :], in_=p_[:, 0:GW], func=AF.Copy), [pb_], [vs_b])
                w_, wb_ = wload(f"win{l}", 3 * h + 2, KD)
                for m in range(MS):
                    p_, pb_ = nps()
                    for k in range(KD):
                        pe(lambda e, p_=p_, w_=w_, m=m, k=k: e.matmul(p_[:, 0:GW], lhsT=hnT[:, k, m * 128:(m + 1) * 128], rhs=w_[:, k, :], start=(k == 0), stop=(k == KD - 1)),
                           [hn_b, wb_], [pb_])
                    act(lambda e, p_=p_, m=m: e.activation(out=gs[:, m, :], in_=p_[:, 0:GW], func=AF.Silu), [pb_], [gs_b])
                for m in range(MS):
                    msl = slice(m * 128, (m + 1) * 128)
                    i2 = m % 2
                    pS, pSb = nps()
                    pe(lambda e, pS=pS, msl=msl: e.matmul(pS[:, 0:128], lhsT=qTs[:, 2, msl], rhs=qTs[:, 0, msl], start=True, stop=True), [qT_b], [pSb])
                    dve(lambda e, pS=pS, i2=i2, h=h: e.tensor_tensor(out=sTs[i2][:], in0=pS[:, 0:128], in1=maskT[:, h, :], op=ALU.mult), [pSb, con_b], [sT_b[i2]])
                    pO, pOb = nps()
                    pe(lambda e, pO=pO, i2=i2, m=m: e.matmul(pO[:, 0:256], lhsT=sTs[i2][:], rhs=vs[:, m, :], start=True, stop=False), [sT_b[i2], vs_b], [pOb])
                    pe(lambda e, pO=pO, msl=msl, h=h: e.matmul(pO[:, 0:256], lhsT=qTs[:, 1, msl], rhs=Sbf[:, h, :], start=False, stop=True), [qT_b, S_b[h]], [pOb])
                    pD, pDb = nps()
                    pe(lambda e, pD=pD, m=m: e.matmul(pD[:, 0:256], lhsT=kds[:, m, :], rhs=vs[:, m, :], start=True, stop=True), [kds_b, vs_b], [pDb])
                    dve(lambda e, pD=pD, h=h: e.scalar_tensor_tensor(out=Sst[:, h, :], in0=Sst[:, h, :], scalar=gC[:, h:h + 1], in1=pD[:, 0:256], op0=ALU.mult, op1=ALU.add),
                        [pDb, con_b, S_b[h]], [S_b[h]])
                    act(lambda e, h=h: e.activation(out=Sbf[:, h, :], in_=Sst[:, h, :], func=AF.Copy), [S_b[h]], [S_b[h]])
                    yr, yrb_ = yrs[i2], yr_b[i2]
                    dve(lambda e, pO=pO: e.bn_stats(out=bst[:, 0:6], in_=pO[:, 0:256]), [pOb], [bst_b])
                    dve(lambda e: e.bn_aggr(out=small[:, 0:2], in_=bst[:, 0:6]), [bst_b], [small_b])
                    act(lambda e: e.activation(out=small[:, 2:3], in_=small[:, 1:2], func=AF.Sqrt, bias=float(EPS), scale=1.0), [small_b], [small_b])
                    dve(lambda e: e.reciprocal(out=small[:, 2:3], in_=small[:, 2:3]), [small_b], [small_b])
                    dve(lambda e, pO=pO, yr=yr: e.tensor_scalar(out=yr[:], in0=pO[:, 0:256], scalar1=small[:, 0:1], scalar2=small[:, 2:3], op0=ALU.subtract, op1=ALU.mult),
                        [pOb, small_b], [yrb_])
                    dve(lambda e, yr=yr, i2=i2, m=m: e.tensor_tensor(out=yrb[i2][:], in0=yr[:], in1=gs[:, m, :], op=ALU.mult), [yrb_, gs_b], [yrb_b[i2]])
                    pt, ptb = nps(6, 8)
                    ptv = pt[:].bitcast(BF16)
                    for jj in range(2):
                        pe(lambda e, ptv=ptv, i2=i2, jj=jj: e.transpose(out=ptv[:, jj * 128:(jj + 1) * 128], in_=yrb[i2][:, jj * 128:(jj + 1) * 128], identity=ident[:]),
                           [yrb_b[i2], con_b], [ptb])
                    for jj in range(2):
                        act(lambda e, ptv=ptv, jj=jj, h=h, msl=msl: e.activation(out=yT[:, 2 * h + jj, msl], in_=ptv[:, jj * 128:(jj + 1) * 128], func=AF.Identity, scale=rngs[:, l, 2 * h + jj:2 * h + jj + 1]), [ptb, con_b], [y_b])
            ssq, ssqb = pst[7], ps_b[7]
            for n in range(NBL):
                w_, wb_ = wload(f"win{l}", 3 * H + n, KD)
                pG, pGb = nps(); pX, pXb = nps()
                for k in range(KD):
                    pe(lambda e, pG=pG, w_=w_, k=k: e.matmul(pG[:, 0:TT], lhsT=w_[:, k, 0:128], rhs=hnT[:, k, :], start=(k == 0), stop=(k == KD - 1)), [hn_b, wb_], [pGb])
                for k in range(KD):
                    pe(lambda e, pX=pX, w_=w_, k=k: e.matmul(pX[:, 0:TT], lhsT=w_[:, k, 128:256], rhs=hnT[:, k, :], start=(k == 0), stop=(k == KD - 1)), [hn_b, wb_], [pXb])
                tg, tgb = ntmp(); tu, tub = ntmp()
                act(lambda e, pG=pG, tg=tg: e.activation(out=tg[:], in_=pG[:, 0:TT], func=AF.Square), [pGb], [tgb])
                dve(lambda e, tg=tg: e.tensor_scalar(out=tg[:], in0=tg[:], scalar1=0.044715, scalar2=1.0, op0=ALU.mult, op1=ALU.add), [tgb], [tgb])
                dve(lambda e, tg=tg, pG=pG: e.tensor_tensor(out=tg[:], in0=tg[:], in1=pG[:, 0:TT], op=ALU.mult), [tgb, pGb], [tgb])
                act(lambda e, tg=tg: e.activation(out=tg[:], in_=tg[:], func=AF.Sigmoid, scale=float(2.0 * np.sqrt(2.0 / np.pi))), [tgb], [tgb])
                dve(lambda e, tg=tg, pG=pG: e.tensor_tensor(out=tg[:], in0=tg[:], in1=pG[:, 0:TT], op=ALU.mult), [tgb, pGb], [tgb])
                lx_, lxb_ = lxt[n % 2], lxt_b[n % 2]
                dve(lambda e, lx_=lx_, n=n: e.tensor_copy(out=lx_[:, 0:3], in_=halo[:, n, :]), [halo_b[n]], [lxb_])
                act(lambda e, pX=pX, lx_=lx_: e.activation(out=lx_[:, 3:3 + TT], in_=pX[:, 0:TT], func=AF.Copy), [pXb], [lxb_])
                xcv, xcvb = ntmp()
                act(lambda e, xcv=xcv, lx_=lx_, n=n, l=l: e.activation(out=xcv[:], in_=lx_[:, 3:3 + TT], func=AF.Identity, bias=lpar["convb"][:, l, n:n + 1], scale=cw[:, l, 3, n:n + 1]),
                    [lxb_, lpar_b, con_b], [xcvb])
                for j in range(3):
                    dve(lambda e, xcv=xcv, lx_=lx_, n=n, j=j, l=l: e.scalar_tensor_tensor(out=xcv[:], in0=lx_[:, j:j + TT], scalar=cw[:, l, j, n:n + 1], in1=xcv[:], op0=ALU.mult, op1=ALU.add),
                        [lxb_, con_b, xcvb], [xcvb])
                dve(lambda e, lx_=lx_, n=n: e.tensor_copy(out=halo[:, n, :], in_=lx_[:, TT:TT + 3]), [lxb_], [halo_b[n]])
                i2 = n % 2
                act(lambda e, xcv=xcv, i2=i2: e.activation(out=sqb[i2][:], in_=xcv[:], func=AF.Copy), [xcvb], [sq_b[i2]])
                pR, pRb = nps(); pI, pIb = nps()
                pe(lambda e, pR=pR, n=n, i2=i2: e.matmul(pR[:, 0:TT], lhsT=gwa[:, n, :], rhs=sqb[i2][:], start=True, stop=True), [gw_b, sq_b[i2]], [pRb])
                pe(lambda e, pI=pI, n=n, i2=i2: e.matmul(pI[:, 0:TT], lhsT=gwx[:, n, :], rhs=sqb[i2][:], start=True, stop=True), [gw_b, sq_b[i2]], [pIb])
                ta, tab = ntmp(); ti, tib = ntmp()
                act(lambda e, pR=pR, ta=ta, n=n, l=l: e.activation(out=ta[:], in_=pR[:, 0:TT], func=AF.Sigmoid, bias=lpar["gab"][:, l, n:n + 1], scale=1.0), [pRb, lpar_b], [tab])
                act(lambda e, pI=pI, ti=ti, n=n, l=l: e.activation(out=ti[:], in_=pI[:, 0:TT], func=AF.Sigmoid, bias=lpar["gxb"][:, l, n:n + 1], scale=1.0), [pIb, lpar_b], [tib])
                dve(lambda e, ti=ti, xcv=xcv: e.tensor_tensor(out=ti[:], in0=ti[:], in1=xcv[:], op=ALU.mult), [tib, xcvb], [tib])
                act(lambda e, ta=ta, tu=tu, n=n, l=l: e.activation(out=tu[:], in_=ta[:], func=AF.Exp, scale=cneg2[:, l, n:n + 1]), [tab, lpar_b], [tub])
                act(lambda e, ta=ta, n=n, l=l: e.activation(out=ta[:], in_=ta[:], func=AF.Exp, scale=cneg[:, l, n:n + 1]), [tab, lpar_b], [tab])
                dve(lambda e, tu=tu: e.tensor_scalar(out=tu[:], in0=tu[:], scalar1=-1.0, scalar2=1.0, op0=ALU.mult, op1=ALU.add), [tub], [tub])
                dve(lambda e, tu=tu: e.tensor_scalar(out=tu[:], in0=tu[:], scalar1=0.0, scalar2=None, op0=ALU.max), [tub], [tub])
                act(lambda e, tu=tu: e.activation(out=tu[:], in_=tu[:], func=AF.Sqrt), [tub], [tub])
                dve(lambda e, tu=tu, ti=ti: e.tensor_tensor(out=tu[:], in0=tu[:], in1=ti[:], op=ALU.mult), [tub, tib], [tub])
                dve(lambda e, ta=ta, tu=tu, ti=ti, n=n: e.tensor_tensor_scan(out=ti[:], data0=ta[:], data1=tu[:], initial=hst[:, n:n + 1], op0=ALU.mult, op1=ALU.add),
                    [tab, tub, h_b[n]], [tib])
                dve(lambda e, ti=ti, n=n: e.tensor_copy(out=hst[:, n:n + 1], in_=ti[:, TT - 1:TT]), [tib], [h_b[n]])
                act(lambda e, ti=ti, i2=i2: e.activation(out=sqb[i2][:], in_=ti[:], func=AF.Square), [tib], [sq_b[i2]])
                pe(lambda e, i2=i2, n=n: e.matmul(ssq[:, 0:TT], lhsT=ones[:], rhs=sqb[i2][:], start=(n == 0), stop=(n == NBL - 1)), [sq_b[i2], con_b], [ssqb])
                dve(lambda e, ti=ti, tg=tg, n=n, l=l: e.scalar_tensor_tensor(out=yT[:, 2 * H + n, :], in0=ti[:], scalar=lpar["lng"][:, l, n:n + 1], in1=tg[:], op0=ALU.mult, op1=ALU.mult),
                    [tib, tgb, lpar_b], [y_b])
            act(lambda e: e.activation(out=rstd[:], in_=ssq[:, 0:TT], func=AF.Sqrt, bias=float(EPS), scale=float(1.0 / LW)), [ssqb], [rstd_b])
            dve(lambda e: e.reciprocal(out=rstd[:], in_=rstd[:]), [rstd_b], [rstd_b])
            for n in range(NBL):
                dve(lambda e, n=n: e.tensor_tensor(out=yT[:, 2 * H + n, :], in0=yT[:, 2 * H + n, :], in1=rstd[:], op=ALU.mult), [rstd_b, y_b], [y_b])
            stat = (pst[6], ps_b[6])
            pend = None
            for g in range(DG):
                w_, wb_ = wload(f"wout{l}", g, MKC)
                for j in range(2):
                    p_, pb_ = nps()
                    for k in range(MKC):
                        pe(lambda e, p_=p_, w_=w_, j=j, k=k: e.matmul(p_[:, 0:TT], lhsT=w_[:, k, j * 128:(j + 1) * 128], rhs=yT[:, k, :], start=(k == 0), stop=(k == MKC - 1)),
                           [y_b, wb_], [pb_])
                    if pend is not None:
                        pend()
                    pend = resid_update(t, 2 * g + j, p_, pb_, 2 * KD, stats=stat)
            if pend is not None:
                pend()
            norm_to_hn(t, 4 * KD, 3 * KD, stats=stat, router=moe)
            if not moe:
                for half in range(2):
                    for gg in range(FH // 2):
                        g = half * (FH // 2) + gg
                        wg_, wgb = wload(f"wg{l}", g, KD)
                        wu_, wub = wload(f"wu{l}", g, KD)
                        for j in range(2):
                            pg, pgb = nps(); pu, pub = nps()
                            for k in range(KD):
                                pe(lambda e, pg=pg, wg_=wg_, j=j, k=k: e.matmul(pg[:, 0:TT], lhsT=wg_[:, k, j * 128:(j + 1) * 128], rhs=hnT[:, k, :], start=(k == 0), stop=(k == KD - 1)), [hn_b, wgb], [pgb])
                            for k in range(KD):
                                pe(lambda e, pu=pu, wu_=wu_, j=j, k=k: e.matmul(pu[:, 0:TT], lhsT=wu_[:, k, j * 128:(j + 1) * 128], rhs=hnT[:, k, :], start=(k == 0), stop=(k == KD - 1)), [hn_b, wub], [pub])
                            t_, tb = ntmp()
                            act(lambda e, pg=pg, t_=t_: e.activation(out=t_[:], in_=pg[:, 0:TT], func=AF.Silu), [pgb], [tb])
                            dve(lambda e, pu=pu, t_=t_, c=2 * gg + j: e.tensor_tensor(out=yT[:, c, :], in0=t_[:], in1=pu[:, 0:TT], op=ALU.mult), [pub, tb], [y_b])
                    for g in range(DG):
                        w_, wb_ = wload(f"wd{l}", half * DG + g, FH)
                        for j in range(2):
                            p_, pb_ = nps()
                            for k in range(FH):
                                pe(lambda e, p_=p_, w_=w_, j=j, k=k: e.matmul(p_[:, 0:TT], lhsT=w_[:, k, j * 128:(j + 1) * 128], rhs=yT[:, k, :], start=(k == 0), stop=(k == FH - 1)), [y_b, wb_], [pb_])
                            resid_update(t, 2 * g + j, p_, pb_, 5 * KD)
            else:
                for m in range(MS):
                    pl, plb = nps(0, 6)
                    pe(lambda e, pl=pl, m=m: e.transpose(out=pl[:, 0:E], in_=lgT[:, m * 128:(m + 1) * 128], identity=identf[0:E, 0:E]), [lgT_b, con_b], [plb])
                    lg = small[:, 16:16 + E]; m1 = small[:, 32:33]; m2 = small[:, 33:34]; eq1 = small[:, 40:40 + E]; w1 = small[:, 34:35]; w2 = small[:, 35:36]
                    dve(lambda e, pl=pl: e.tensor_copy(out=small[:, 16:16 + E], in_=pl[:, 0:E]), [plb], [small_b])
                    dve(lambda e: e.tensor_reduce(out=small[:, 32:33], in_=small[:, 16:16 + E], axis=mybir.AxisListType.X, op=ALU.max), [small_b], [small_b])
                    dve(lambda e: e.tensor_scalar(out=small[:, 40:40 + E], in0=small[:, 16:16 + E], scalar1=small[:, 32:33], scalar2=None, op0=ALU.is_ge), [small_b], [small_b])
                    dve(lambda e: e.scalar_tensor_tensor(out=small[:, 48:48 + E], in0=small[:, 40:40 + E], scalar=-1e30, in1=small[:, 16:16 + E], op0=ALU.mult, op1=ALU.add), [small_b], [small_b])
                    dve(lambda e: e.tensor_reduce(out=small[:, 33:34], in_=small[:, 48:48 + E], axis=mybir.AxisListType.X, op=ALU.max), [small_b], [small_b])
                    dve(lambda e: e.tensor_scalar(out=small[:, 56:56 + E], in0=small[:, 48:48 + E], scalar1=small[:, 33:34], scalar2=None, op0=ALU.is_ge), [small_b], [small_b])
                    dve(lambda e: e.tensor_tensor(out=small[:, 34:35], in0=small[:, 32:33], in1=small[:, 33:34], op=ALU.subtract), [small_b], [small_b])
                    act(lambda e: e.activation(out=small[:, 34:35], in_=small[:, 34:35], func=AF.Sigmoid), [small_b], [small_b])
                    dve(lambda e: e.tensor_scalar(out=small[:, 35:36], in0=small[:, 34:35], scalar1=-1.0, scalar2=1.0, op0=ALU.mult, op1=ALU.add), [small_b], [small_b])
                    dve(lambda e, m=m: e.tensor_scalar(out=comb[:, m, :], in0=small[:, 40:40 + E], scalar1=small[:, 34:35], scalar2=None, op0=ALU.mult), [small_b], [comb_b])
                    dve(lambda e, m=m: e.scalar_tensor_tensor(out=comb[:, m, :], in0=small[:, 56:56 + E], scalar=small[:, 35:36], in1=comb[:, m, :], op0=ALU.mult, op1=ALU.add), [small_b, comb_b], [comb_b])
                    pc, pcb = nps(6, 8)
                    pe(lambda e, pc=pc, m=m: e.transpose(out=pc[0:E, 0:128], in_=comb[:, m, :], identity=identf[:]), [comb_b, con_b], [pcb])
                    dve(lambda e, pc=pc, m=m: e.tensor_copy(out=combT[:, m * 128:(m + 1) * 128], in_=pc[0:E, 0:128]), [pcb], [combT_b])
                NG2 = DE // GW
                for qtr in range(NQ):
                    for el in range(EQ):
                        ex = qtr * EQ + el
                        pb2, pb2b = nps(6, 8)
                        pe(lambda e, pb2=pb2, ex=ex: e.matmul(pb2[:, 0:TT], lhsT=sel[:, ex, :], rhs=combT[:, :], start=True, stop=True), [sel_b, combT_b], [pb2b])
                        dve(lambda e, pb2=pb2: e.tensor_copy(out=combB[:], in_=pb2[:, 0:TT]), [pb2b], [combB_b])
                        for gg in range(NG2):
                            wg_, wgb = wload(f"wg{l}", ex * NG2 + gg, KD)
                            wu_, wub = wload(f"wu{l}", ex * NG2 + gg, KD)
                            for j in range(2):
                                pg, pgb = nps(); pu, pub = nps()
                                for k in range(KD):
                                    pe(lambda e, pg=pg, wg_=wg_, j=j, k=k: e.matmul(pg[:, 0:TT], lhsT=wg_[:, k, j * 128:(j + 1) * 128], rhs=hnT[:, k, :], start=(k == 0), stop=(k == KD - 1)), [hn_b, wgb], [pgb])
                                for k in range(KD):
                                    pe(lambda e, pu=pu, wu_=wu_, j=j, k=k: e.matmul(pu[:, 0:TT], lhsT=wu_[:, k, j * 128:(j + 1) * 128], rhs=hnT[:, k, :], start=(k == 0), stop=(k == KD - 1)), [hn_b, wub], [pub])
                                t_, tb = ntmp()
                                act(lambda e, pg=pg, t_=t_: e.activation(out=t_[:], in_=pg[:, 0:TT], func=AF.Silu), [pgb], [tb])
                                dve(lambda e, t_=t_, ex=ex: e.tensor_tensor(out=t_[:], in0=t_[:], in1=combB[:], op=ALU.mult), [tb, combB_b], [tb])
                                dve(lambda e, pu=pu, t_=t_, c=el * DEC + 2 * gg + j: e.tensor_tensor(out=yT[:, c, :], in0=t_[:], in1=pu[:, 0:TT], op=ALU.mult), [pub, tb], [y_b])
                    for g in range(DG):
                        w_, wb_ = wload(f"wd{l}", qtr * DG + g, QC)
                        for j in range(2):
                            p_, pb_ = nps()
                            for k in range(QC):
                                pe(lambda e, p_=p_, w_=w_, j=j, k=k: e.matmul(p_[:, 0:TT], lhsT=w_[:, k, j * 128:(j + 1) * 128], rhs=yT[:, k, :], start=(k == 0), stop=(k == QC - 1)), [y_b, wb_], [pb_])
                            resid_update(t, 2 * g + j, p_, pb_, 5 * KD)

    out_b = Buf("out")
    for t in range(NT):
        t0 = t * TT
        pst_, pstb = nps(6, 8)
        for k in range(KD):
            x_, xb = nxc()
            load(x_[:], xsrc[k][t][k, :, t0:t0 + TT], xb, xs_bt[k][t])
            i = k % 2
            act(lambda e, x_=x_, i=i: e.activation(out=sqb[i][:], in_=x_[:], func=AF.Square), [xb], [sq_b[i]])
            pe(lambda e, i=i, k=k, pst_=pst_: e.matmul(pst_[:, 0:TT], lhsT=ones[:], rhs=sqb[i][:], start=(k == 0), stop=(k == KD - 1)), [sq_b[i], con_b], [pstb])
        act(lambda e, pst_=pst_: e.activation(out=rstd[:], in_=pst_[:, 0:TT], func=AF.Sqrt, bias=float(EPS), scale=float(1.0 / D)), [pstb], [rstd_b])
        dve(lambda e: e.reciprocal(out=rstd[:], in_=rstd[:]), [rstd_b], [rstd_b])
        for k in range(KD):
            x_, xb = nxc()
            load(x_[:], xsrc[k][t][k, :, t0:t0 + TT], xb, xs_bt[k][t])
            dve(lambda e, x_=x_, k=k: e.scalar_tensor_tensor(out=x_[:], in0=x_[:], scalar=fngs[:, k:k + 1], in1=rstd[:], op0=ALU.mult, op1=ALU.mult), [xb, rstd_b, con_b], [xb])
            xstore(outT[k, :, t0:t0 + TT], x_[:], xb, out_b)

    S.emit(nc, st)
    st.close()
    return nc


def _tile_w(w, gw=GW):
    K, N = w.shape
    return np.ascontiguousarray(w.reshape(K // 128, 128, N // gw, gw).transpose(2, 1, 0, 3))


def _pp(v, nb):
    v = np.asarray(v)
    lead = v.shape[:-1]
    return np.ascontiguousarray(np.moveaxis(v.reshape(*lead, nb, 128), -1, 0))


def make_inputs(cfg, inp):
    D, H, LW, FF, E, DE, L, SEQ, TT, B = (cfg[k] for k in ("D", "H", "LW", "FF", "E", "DE", "L", "SEQ", "TT", "B"))
    KD = D // 128; Vw = H * 256; NBL = LW // 128; QK = H * 128; NCH = SEQ // 128
    f32 = np.float32
    shared = {}
    aw = np.asarray(inp["ada_w"], f32)
    shared["ada_w"] = np.ascontiguousarray(aw.reshape(KD, 128, 6 * KD, 128).transpose(2, 1, 0, 3))
    shared["ada_b"] = _pp(np.asarray(inp["ada_b"], f32), 6 * KD)
    shared["ada_tab"] = _pp(np.asarray(inp["ada_table"], f32).reshape(L, 6 * D), 6 * KD)
    shared["convw"] = _pp(np.asarray(inp["conv_w"], f32), NBL)
    for n, k in (("convb", "conv_b"), ("gab", "gate_a_b"), ("gxb", "gate_x_b"), ("lam", "lru_lambda"), ("lng", "lru_norm_g")):
        shared[n] = _pp(np.asarray(inp[k], f32), NBL)
    shared["rng"] = _pp(np.asarray(inp["ret_norm_g"], f32), 2 * H)
    shared["fng"] = _pp(np.asarray(inp["final_norm_g"], f32), KD)
    shared["gaw"] = np.ascontiguousarray(np.asarray(inp["gate_a_w"], f32).transpose(2, 0, 1, 3))
    shared["gxw"] = np.ascontiguousarray(np.asarray(inp["gate_x_w"], f32).transpose(2, 0, 1, 3))
    rw = np.asarray(inp["router_w"], f32)
    shared["router"] = np.ascontiguousarray(rw.reshape(rw.shape[0], KD, 128, E).transpose(2, 0, 1, 3))
    lg = np.log1p(-np.exp2(-5.0 - np.arange(H, dtype=np.float64)))
    idx = np.arange(128, dtype=np.float64)
    rel = idx[None, :] - idx[:, None]
    maskT = np.where(rel[None] >= 0, np.exp(lg[:, None, None] * np.maximum(rel[None], 0)), 0.0) * (128 ** -0.5)
    shared["maskT"] = np.ascontiguousarray(maskT.transpose(1, 0, 2)).astype(f32)
    shared["qdec"] = np.ascontiguousarray(np.broadcast_to(np.exp(lg[:, None] * (idx[None, :] + 1.0))[None], (128, H, 128))).astype(f32)
    shared["kdec"] = np.ascontiguousarray((np.exp(lg[None, :] * (127.0 - idx[:, None])) * (128 ** -0.5))).astype(f32)
    shared["gC"] = np.ascontiguousarray(np.broadcast_to(np.exp(lg * 128.0)[None], (128, H))).astype(f32)
    invf = np.exp2(-np.arange(0, 128, 2, dtype=f32) / f32(128) * np.log2(f32(10000.0))).astype(f32)
    shared["invf"] = np.ascontiguousarray(np.broadcast_to(invf[None], (128, 64)))
    shared["ident"] = np.eye(128, dtype=f32)
    S0, S1, S2, S3, S4 = QK, 2 * QK, 2 * QK + Vw, 2 * QK + 2 * Vw, 2 * QK + 2 * Vw + LW
    for l in range(L):
        w = np.asarray(inp["w_in"][l], f32)
        cols = []
        for h in range(H):
            cols += [w[:, h * 128:(h + 1) * 128], w[:, S0 + h * 128:S0 + (h + 1) * 128], w[:, S1 + h * 256:S1 + (h + 1) * 256], w[:, S2 + h * 256:S2 + (h + 1) * 256]]
        for n in range(NBL):
            cols += [w[:, S3 + n * 128:S3 + (n + 1) * 128], w[:, S4 + n * 128:S4 + (n + 1) * 128]]
        shared[f"win{l}"] = _tile_w(np.concatenate(cols, axis=1))
        shared[f"wout{l}"] = _tile_w(np.asarray(inp["w_out"][l], f32))
        if l % 2 == 0:
            i = l // 2
            shared[f"wg{l}"] = _tile_w(np.asarray(inp["ffn_w_gate"][i], f32)); shared[f"wu{l}"] = _tile_w(np.asarray(inp["ffn_w_up"][i], f32))
            wd = np.asarray(inp["ffn_w_down"][i], f32)
            shared[f"wd{l}"] = np.concatenate([_tile_w(wd[hh * FF // 2:(hh + 1) * FF // 2]) for hh in range(2)], axis=0)
        else:
            i = l // 2
            mg = np.asarray(inp["moe_w_gate"][i], f32); mu = np.asarray(inp["moe_w_up"][i], f32); md = np.asarray(inp["moe_w_down"][i], f32)
            shared[f"wg{l}"] = np.concatenate([_tile_w(mg[e]) for e in range(E)], axis=0)
            shared[f"wu{l}"] = np.concatenate([_tile_w(mu[e]) for e in range(E)], axis=0)
            mdf = md.reshape(E * DE, D)
            q = E * DE // 4
            shared[f"wd{l}"] = np.concatenate([_tile_w(mdf[qq * q:(qq + 1) * q]) for qq in range(4)], axis=0)
    maps = []
    x = np.asarray(inp["x"], f32); c = np.asarray(inp["c"], f32); pos = np.asarray(inp["positions"], np.int32)
    for b in range(B):
        m = dict(shared)
        m["xT"] = np.ascontiguousarray(x[b].T.reshape(KD, 128, SEQ))
        m["cT"] = _pp(c[b], KD)
        m["pos"] = np.ascontiguousarray(pos[b].reshape(NCH, 128).T)
        maps.append(m)
    return maps


def run(cfg, inp):
    nc = build(cfg)
    maps = make_inputs(cfg, inp)
    B = cfg["B"]
    res = run_bass_kernel_spmd(nc, maps, core_ids=list(range(B)))
    D, SEQ = cfg["D"], cfg["SEQ"]
    out = np.stack([np.asarray(res.results[b]["outT"]).reshape(D, SEQ).T for b in range(B)], axis=0)
    return np.ascontiguousarray(out.astype(np.float32))


def kernel(**inputs):
    return run(FULL, inputs)
```

```python
import contextlib
import numpy as np
import concourse.bass as bass
import concourse.mybir as mybir
from concourse.bass_utils import run_bass_kernel_spmd

ALU = mybir.AluOpType
AF = mybir.ActivationFunctionType
F32, BF16, I32 = mybir.dt.float32, mybir.dt.bfloat16, mybir.dt.int32
GW = 256
EPS = 1e-6
SAME_ENG_SYNC = True
STORE_ENG = "pool"

FULL = dict(D=4096, H=8, LW=2048, FF=8192, E=8, DE=1792, L=4, SEQ=8192, TT=512, B=2)


class Buf:
    __slots__ = ("name", "w", "rs", "cnt", "sem")

    def __init__(self, name):
        self.name = name; self.w = None; self.rs = []; self.cnt = 0; self.sem = None


class Sched:
    ENGS = ("pe", "act", "dve", "pool", "sp")

    def __init__(self):
        self.q = {e: [] for e in self.ENGS}
        self.dmabufs = []

    def _deps(self, rd, wr):
        deps = []
        for b in rd:
            if b.w is not None:
                deps.append(b.w)
        for b in wr:
            if b.w is not None:
                deps.append(b.w)
            deps.extend(b.rs)
        return deps

    def op(self, eng, fn, rd=(), wr=()):
        deps = self._deps(rd, wr)
        ev = ("e", eng, len(self.q[eng]))
        self.q[eng].append([fn, deps, False, None])
        for b in rd:
            b.rs.append(ev)
        for b in wr:
            b.w = ev; b.rs = []
        return ev

    def dma(self, eng, fn, src, dst, n=1, slot=None):
        deps = self._deps([src], [dst])
        if slot is None:
            slot = dst
        elif slot.w is not None:
            deps.append(slot.w)
        if slot.sem is None:
            slot.sem = True; self.dmabufs.append(slot)
        slot.cnt += 16 * n
        ev = ("d", slot, slot.cnt)
        if slot is not dst:
            slot.w = ev
        self.q[eng].append([fn, deps, False, slot])
        src.rs.append(ev)
        dst.w = ev; dst.rs = []
        return ev

    def emit(self, nc, stack):
        for e in self.ENGS:
            for ins in self.q[e]:
                for d in ins[1]:
                    if d[0] == "e":
                        if d[1] == "pe" and e == "pe":
                            continue
                        if d[1] == e and not SAME_ENG_SYNC:
                            continue
                        self.q[d[1]][d[2]][2] = True
        cum = {}
        for e in self.ENGS:
            c = 0; arr = []
            for ins in self.q[e]:
                if ins[2]:
                    c += 1
                arr.append(c)
            cum[e] = arr
        esem = {e: stack.enter_context(nc.semaphore("es_" + e)) for e in self.ENGS}
        for b in self.dmabufs:
            b.sem = stack.enter_context(nc.semaphore("ds_" + b.name))
        engobj = {"pe": "tensor", "act": "scalar", "dve": "vector", "pool": "gpsimd", "sp": "sync"}
        block = stack.enter_context(nc.Block())
        finals = [(b.sem, b.cnt) for b in self.dmabufs]

        def run(e):
            def body(eng):
                waited = {}
                for idx, ins in enumerate(self.q[e]):
                    need = {}
                    for d in ins[1]:
                        if d[0] == "e":
                            if d[1] == "pe" and e == "pe":
                                continue
                            if d[1] == e and not SAME_ENG_SYNC:
                                continue
                            key = ("e", d[1]); sem = esem[d[1]]; val = cum[d[1]][d[2]]
                        else:
                            key = ("d", id(d[1])); sem = d[1].sem; val = d[2]
                        if val > need.get(key, (None, 0))[1]:
                            need[key] = (sem, val)
                    for key, (sem, val) in need.items():
                        if waited.get(key, 0) >= val:
                            continue
                        waited[key] = val
                        eng.wait_ge(sem, val)
                    if ins[3] is not None:
                        ins[0](eng, ins[3].sem)
                    else:
                        r = ins[0](eng)
                        if ins[2]:
                            r.then_inc(esem[e], 1)
                if e == "sp":
                    for sem, cnt in finals:
                        eng.wait_ge(sem, cnt)
            return body
        for e in self.ENGS:
            getattr(block, engobj[e])(run(e))


def build(cfg):
    D, H, LW, FF, E, DE, L, SEQ, TT = (cfg[k] for k in ("D", "H", "LW", "FF", "E", "DE", "L", "SEQ", "TT"))
    KD = D // 128; Vw = H * 256; NBL = LW // 128; MIX = Vw + LW; MKC = MIX // 128
    NT = SEQ // TT; MS = TT // 128; NCH = SEQ // 128
    NBI = (2 * H * 128 + 2 * Vw + 2 * LW) // GW
    DG = D // GW
    FH = FF // 128 // 2
    NQ = 4; EQ = E // NQ; DEC = DE // 128; QC = EQ * DEC
    nc = bass.Bass("TRN2", target_bir_lowering=False)
    S = Sched()
    st = contextlib.ExitStack()

    def din(name, shape, dt=F32):
        return nc.dram_tensor(name, list(shape), dt, kind="ExternalInput").ap()

    def dint(name, shape, dt):
        return nc.dram_tensor(name, list(shape), dt, kind="Internal").ap()

    xT_in = din("xT", [KD, 128, SEQ])
    outT = nc.dram_tensor("outT", [KD, 128, SEQ], F32, kind="ExternalOutput").ap()
    xs = dint("xs", [KD, 128, SEQ], F32)
    cT = din("cT", [128, KD]); pos = din("pos", [128, NCH], I32)
    ada_w = din("ada_w", [6 * KD, 128, KD, 128]); ada_b = din("ada_b", [128, 6 * KD]); ada_tab = din("ada_tab", [128, L, 6 * KD])
    convw = din("convw", [128, L, 4, NBL])
    lp = {n: din(n, [128, L, NBL]) for n in ("convb", "gab", "gxb", "lam", "lng")}
    rng = din("rng", [128, L, 2 * H]); fng = din("fng", [128, KD])
    gaw = din("gaw", [128, L, NBL, 128]); gxw = din("gxw", [128, L, NBL, 128])
    router = din("router", [128, max(L // 2, 1), KD, E])
    maskT_d = din("maskT", [128, H, 128]); qdec_d = din("qdec", [128, H, 128]); kdec_d = din("kdec", [128, H]); gC_d = din("gC", [128, H])
    invf_d = din("invf", [128, 64]); ident_d = din("ident", [128, 128])

    wspec = {}
    for l in range(L):
        wspec[f"win{l}"] = [NBI, 128, KD, GW]
        wspec[f"wout{l}"] = [DG, 128, MKC, GW]
        if l % 2 == 0:
            wspec[f"wg{l}"] = [FF // GW, 128, KD, GW]; wspec[f"wu{l}"] = [FF // GW, 128, KD, GW]
            wspec[f"wd{l}"] = [2 * DG, 128, FH, GW]
        else:
            wspec[f"wg{l}"] = [E * DE // GW, 128, KD, GW]; wspec[f"wu{l}"] = [E * DE // GW, 128, KD, GW]
            wspec[f"wd{l}"] = [NQ * DG, 128, QC, GW]
    wf, wb, wbuf = {}, {}, {}
    for n, shp in wspec.items():
        wf[n] = din(n, shp); wb[n] = dint(n + "_b", shp, BF16); wbuf[n] = Buf(n)
    ext = Buf("ext")

    def sb(name, shape, dt=F32):
        return st.enter_context(nc.sbuf_tensor("sb_" + name, list(shape), dt))

    def ps(name):
        return st.enter_context(nc.psum_tensor(name, [128, 512], F32))

    NWT = 3
    wt = [sb(f"wt{i}", [128, 32, GW], BF16) for i in range(NWT)]; wt_b = [Buf(f"wt{i}") for i in range(NWT)]
    hnT = sb("hnT", [128, KD, TT], BF16); hn_b = Buf("hnT")
    yT = sb("yT", [128, max(MKC, FH, QC), TT], BF16); y_b = Buf("yT")
    NXC = 3
    xc = [sb(f"xc{i}", [128, TT]) for i in range(NXC)]; xc_b = [Buf(f"xc{i}") for i in range(NXC)]
    NTMP = 4
    tmp = [sb(f"tmp{i}", [128, TT]) for i in range(NTMP)]; tmp_b = [Buf(f"tmp{i}") for i in range(NTMP)]
    sqb = [sb(f"sq{i}", [128, TT], BF16) for i in range(2)]; sq_b = [Buf(f"sq{i}") for i in range(2)]
    xcb = [sb(f"xcb{i}", [128, TT], BF16) for i in range(2)]; xcb_b = [Buf(f"xcb{i}") for i in range(2)]
    ltmp = [sb(f"ltmp{i}", [128, TT]) for i in range(4)]; ltmp_b = [Buf(f"ltmp{i}") for i in range(4)]
    rstd = sb("rstd", [128, TT]); rstd_b = Buf("rstd")
    cosT = sb("cosT", [128, MS, 64]); sinT = sb("sinT", [128, MS, 64]); rope_b = Buf("rope")
    mod = sb("mod", [128, 6 * KD]); modb = sb("modb", [128, 6 * KD]); mod_b = Buf("mod"); modb_b = Buf("modb")
    tabs_b = Buf("tabs")
    ident = sb("ident", [128, 128], BF16); identf = sb("identf", [128, 128]); ones = sb("ones", [128, 128], BF16); con_b = Buf("consts")
    maskT = sb("maskT", [128, H, 128]); qdec = sb("qdec", [128, H, 128]); kdec = sb("kdec", [128, H]); gC = sb("gC", [128, H])
    Sst = sb("Sst", [128, H, 256]); Sbf = sb("Sbf", [128, H, 256], BF16); S_b = [Buf(f"S{h}") for h in range(H)]
    hst = sb("hst", [128, NBL]); h_b = [Buf(f"h{n}") for n in range(NBL)]
    halo = sb("halo", [128, NBL, 3]); halo_b = [Buf(f"halo{n}") for n in range(NBL)]
    lxt = [sb(f"lxt{i}", [128, 3 + TT]) for i in range(2)]; lxt_b = [Buf(f"lxt{i}") for i in range(2)]
    lpar = {n: sb("s_" + n, [128, L, NBL]) for n in lp}; cw = sb("cw", [128, L, 4, NBL]); cneg = sb("cneg", [128, L, NBL]); cneg2 = sb("cneg2", [128, L, NBL]); lpar_b = Buf("lpar")
    gwa = sb("gwa", [128, NBL, 128], BF16); gwx = sb("gwx", [128, NBL, 128], BF16); gw_b = Buf("gw")
    rngs = sb("rngs", [128, L, 2 * H]); rng_b = con_b
    fngs = sb("fngs", [128, KD])
    rw = sb("rw", [128, KD, E]); rw_b = Buf("rw")
    qks = sb("qks", [128, 2, 128], BF16); qks_b = Buf("qks")
    kds = sb("kds", [128, MS, 128], BF16); kds_b = Buf("kds")
    qTs = sb("qTs", [128, 3, TT], BF16); qT_b = Buf("qTs")
    vs = sb("vs", [128, MS, 256], BF16); vs_b = Buf("vs")
    gs = sb("gs", [128, MS, 256], BF16); gs_b = Buf("gs")
    sTs = [sb(f"sT{i}", [128, 128], BF16) for i in range(2)]; sT_b = [Buf(f"sT{i}") for i in range(2)]
    yrs = [sb(f"yr{i}", [128, 256]) for i in range(2)]; yr_b = [Buf(f"yr{i}") for i in range(2)]
    yrb = [sb(f"yrb{i}", [128, 256], BF16) for i in range(2)]; yrb_b = [Buf(f"yrb{i}") for i in range(2)]
    bst = sb("bst", [128, 8]); bst_b = Buf("bst")
    comb = sb("comb", [128, MS, E]); comb_b = Buf("comb"); combT = sb("combT", [E, TT]); combT_b = Buf("combT")
    sel = sb("sel", [E, E, 128]); combB = sb("combB", [128, TT]); combB_b = Buf("combB"); sel_b = Buf("sel")
    small = sb("small", [128, 64]); small_b = Buf("small")
    lgT = sb("lgT", [E, TT]); lgT_b = Buf("lgT"); cshift = sb("cshift", [E, 1]); cshift_b = Buf("cshift")
    cs = sb("cs", [128, KD]); cs_b = Buf("cs")
    pst = [ps(f"ps{i}") for i in range(8)]; ps_b = [Buf(f"ps{i}") for i in range(8)]
    psrr = [0]

    def nps(lo=0, hi=6):
        i = lo + psrr[0] % (hi - lo); psrr[0] += 1
        return pst[i], ps_b[i]
    trr = [0]

    def ntmp():
        i = trr[0] % NTMP; trr[0] += 1
        return tmp[i], tmp_b[i]
    xrr = [0]

    def nxc():
        i = xrr[0] % NXC; xrr[0] += 1
        return xc[i], xc_b[i]

    def load(dst_ap, src_ap, dbuf, sbuf=ext, eng="sp"):
        S.dma(eng, lambda e, sem: e.dma_start(out=dst_ap, in_=src_ap).then_inc(sem, 16), sbuf, dbuf)

    def dve(fn, rd, wr):
        S.op("dve", fn, rd, wr)

    def act(fn, rd, wr):
        S.op("act", fn, rd, wr)

    def pe(fn, rd, wr):
        S.op("pe", fn, rd, wr)

    def cast_weight(n):
        tot = int(np.prod(wspec[n])); rows = tot // 2048
        src = wf[n].rearrange("a p k c -> (a p k c)").rearrange("(r c) -> r c", c=2048)
        dst = wb[n].rearrange("a p k c -> (a p k c)").rearrange("(r c) -> r c", c=2048)
        step = 4096
        for r0 in range(0, rows, step):
            r1 = min(rows, r0 + step)
            S.dma("pool", lambda e, sem, r0=r0, r1=r1: e.dma_start(out=dst[r0:r1, :], in_=src[r0:r1, :]).then_inc(sem, 16), ext, wbuf[n])
    load(gwa[:], gaw[:, 0], gw_b, eng="pool")
    load(gwx[:], gxw[:, 0], gw_b, eng="pool")
    for l in range(L):
        for n in (f"win{l}", f"wout{l}", f"wg{l}", f"wu{l}", f"wd{l}"):
            cast_weight(n)

    load(identf[:], ident_d, con_b)
    dve(lambda e: e.tensor_copy(out=ident[:], in_=identf[:]), [con_b], [con_b])
    dve(lambda e: e.memset(ones[:], 1.0), [], [con_b])
    for t_, d_ in ((maskT, maskT_d), (qdec, qdec_d), (kdec, kdec_d), (gC, gC_d), (cw, convw), (fngs, fng), (rngs, rng)):
        load(t_[:], d_, con_b)
    for n in lp:
        load(lpar[n][:], lp[n], lpar_b)
    act(lambda e: e.activation(out=cneg[:], in_=lpar["lam"][:], func=AF.Exp, scale=-1.0), [lpar_b], [lpar_b])
    act(lambda e: e.activation(out=cneg[:], in_=cneg[:], func=AF.Ln, bias=1.0, scale=1.0), [lpar_b], [lpar_b])
    dve(lambda e: e.tensor_scalar(out=cneg2[:], in0=cneg[:], scalar1=-16.0, scalar2=None, op0=ALU.mult), [lpar_b], [lpar_b])
    dve(lambda e: e.tensor_scalar(out=cneg[:], in0=cneg[:], scalar1=-8.0, scalar2=None, op0=ALU.mult), [lpar_b], [lpar_b])
    dve(lambda e: e.memset(Sst[:], 0.0), [], S_b)
    dve(lambda e: e.memset(Sbf[:], 0.0), [], S_b)
    dve(lambda e: e.memset(hst[:], 0.0), [], h_b)
    dve(lambda e: e.memset(halo[:], 0.0), [], halo_b)
    dve(lambda e: e.memset(sel[:], 0.0), [], [sel_b])
    dve(lambda e: e.tensor_copy(out=sel[:, :, :], in_=identf[0:E, 0:E].unsqueeze(2).broadcast_to([E, E, 128])), [con_b, sel_b], [sel_b])

    posi = st.enter_context(nc.sbuf_tensor("sb_posi", [128, NCH], I32)); posf = sb("posf", [128, NCH]); invf = sb("invf", [128, 64])
    angb = sb("angb", [128, MS, 64]); angn = sb("angn", [128, MS, 64]); angi = st.enter_context(nc.sbuf_tensor("sb_angi", [128, MS, 64], I32))
    load(posi[:], pos, rope_b); load(invf[:], invf_d, rope_b)
    dve(lambda e: e.tensor_copy(out=posf[:], in_=posi[:]), [rope_b], [rope_b])
    TWO_PI = 2.0 * np.pi; C1 = 6.28125; C2 = TWO_PI - C1

    def rope_tile(t):
        for table, shift in ((sinT, 0.0), (cosT, 0.5 * np.pi)):
            dve(lambda e: e.tensor_tensor(out=angb[:], in0=posf[:, t * MS:(t + 1) * MS].unsqueeze(2).broadcast_to([128, MS, 64]),
                                          in1=invf[:].unsqueeze(1).broadcast_to([128, MS, 64]), op=ALU.mult), [rope_b], [rope_b])
            if shift:
                dve(lambda e, s_=shift: e.tensor_scalar(out=angb[:], in0=angb[:], scalar1=float(s_), scalar2=None, op0=ALU.add), [rope_b], [rope_b])
            dve(lambda e: e.tensor_scalar(out=angn[:], in0=angb[:], scalar1=float(1.0 / TWO_PI), scalar2=None, op0=ALU.mult), [rope_b], [rope_b])
            dve(lambda e: e.tensor_copy(out=angi[:], in_=angn[:]), [rope_b], [rope_b])
            dve(lambda e: e.tensor_copy(out=angn[:], in_=angi[:]), [rope_b], [rope_b])
            dve(lambda e: e.scalar_tensor_tensor(out=angb[:], in0=angn[:], scalar=float(-C1), in1=angb[:], op0=ALU.mult, op1=ALU.add), [rope_b], [rope_b])
            dve(lambda e: e.scalar_tensor_tensor(out=angb[:], in0=angn[:], scalar=float(-C2), in1=angb[:], op0=ALU.mult, op1=ALU.add), [rope_b], [rope_b])
            dve(lambda e: e.tensor_scalar(out=angn[:], in0=angb[:], scalar1=float(np.pi), scalar2=float(-TWO_PI), op0=ALU.is_gt, op1=ALU.mult), [rope_b], [rope_b])
            dve(lambda e: e.tensor_tensor(out=angb[:], in0=angb[:], in1=angn[:], op=ALU.add), [rope_b], [rope_b])
            dve(lambda e: e.tensor_scalar(out=angn[:], in0=angb[:], scalar1=float(-np.pi), scalar2=float(TWO_PI), op0=ALU.is_lt, op1=ALU.mult), [rope_b], [rope_b])
            dve(lambda e: e.tensor_tensor(out=angb[:], in0=angb[:], in1=angn[:], op=ALU.add), [rope_b], [rope_b])
            act(lambda e, t_=table: e.activation(out=t_[:], in_=angb[:], func=AF.Sin), [rope_b], [rope_b])

    load(cs[:], cT, cs_b)
    act(lambda e: e.activation(out=cs[:], in_=cs[:], func=AF.Silu), [cs_b], [cs_b])
    load(modb[:], ada_b, modb_b)
    yTf = yT[:, 0:KD, 0:256].bitcast(F32) if TT >= 256 else None
    adaw_t = [yTf[:, :, 0:128]]; adaw_b = [y_b]
    for g in range(6 * KD):
        i = 0
        load(adaw_t[i], ada_w[g], adaw_b[i])
        p_, pb_ = nps(6, 8)
        for k in range(KD):
            pe(lambda e, p_=p_, i=i, k=k: e.matmul(p_[:, 0:1], lhsT=adaw_t[i][:, k, :], rhs=cs[:, k:k + 1], start=(k == 0), stop=(k == KD - 1)),
               [adaw_b[i], cs_b], [pb_])
        dve(lambda e, p_=p_, g=g: e.tensor_tensor(out=modb[:, g:g + 1], in0=p_[:, 0:1], in1=modb[:, g:g + 1], op=ALU.add), [pb_, modb_b], [modb_b])

    xs_bt = [[Buf(f"xs{k}_{t}") for t in range(NT)] for k in range(KD)]
    xsrc = [[xT_in for t in range(NT)] for k in range(KD)]
    NSLOT = 8
    st_slots = [Buf(f"stslot{i}") for i in range(NSLOT)]
    st_rr = [0]

    def xstore(dst_ap, src_ap, sbuf, dbuf):
        sl = st_slots[st_rr[0] % NSLOT]; st_rr[0] += 1
        S.dma(STORE_ENG, lambda e, sem: e.dma_start(out=dst_ap, in_=src_ap).then_inc(sem, 16), sbuf, dbuf, slot=sl)

    wrr = [0]

    def wload(name, idx, kc):
        i = wrr[0] % NWT; wrr[0] += 1
        S.dma("sp", lambda e, sem, i=i: e.dma_start(out=wt[i][:, 0:kc, :], in_=wb[name][idx]).then_inc(sem, 16), wbuf[name], wt_b[i])
        return wt[i], wt_b[i]

    def norm_to_hn(t, a_off, s_off, stats=None, router=False):
        t0 = t * TT
        if stats is not None:
            pst_, pstb = stats
        else:
            pst_, pstb = nps(6, 8)
            for k in range(KD):
                x_, xb = nxc()
                load(x_[:], xsrc[k][t][k, :, t0:t0 + TT], xb, xs_bt[k][t])
                i = k % 2
                act(lambda e, x_=x_, i=i: e.activation(out=sqb[i][:], in_=x_[:], func=AF.Square), [xb], [sq_b[i]])
                pe(lambda e, i=i, k=k: e.matmul(pst_[:, 0:TT], lhsT=ones[:], rhs=sqb[i][:], start=(k == 0), stop=(k == KD - 1)), [sq_b[i], con_b], [pstb])
        act(lambda e: e.activation(out=rstd[:], in_=pst_[:, 0:TT], func=AF.Sqrt, bias=float(EPS), scale=float(1.0 / D)), [pstb], [rstd_b])
        dve(lambda e: e.reciprocal(out=rstd[:], in_=rstd[:]), [rstd_b], [rstd_b])
        for k in range(KD):
            x_, xb = nxc()
            load(x_[:], xsrc[k][t][k, :, t0:t0 + TT], xb, xs_bt[k][t])
            t_, tb = ntmp()
            dve(lambda e, x_=x_, t_=t_, k=k: e.scalar_tensor_tensor(out=t_[:], in0=x_[:], scalar=mod[:, a_off + k:a_off + k + 1], in1=rstd[:],
                                                                    op0=ALU.mult, op1=ALU.mult), [xb, rstd_b, mod_b], [tb])
            act(lambda e, t_=t_, k=k: e.activation(out=hnT[:, k, :], in_=t_[:], func=AF.Identity, bias=mod[:, s_off + k:s_off + k + 1], scale=1.0),
                [tb, mod_b], [hn_b])
            if router:
                pe(lambda e, t_=t_, k=k: e.matmul(pst[7][0:E, 0:TT], lhsT=rw[:, k, :], rhs=t_[:], start=(k == 0), stop=(k == KD - 1)), [tb, rw_b], [ps_b[7]])
        if router:
            dve(lambda e: e.tensor_scalar(out=lgT[:], in0=pst[7][0:E, 0:TT], scalar1=cshift[:, 0:1], scalar2=None, op0=ALU.add), [ps_b[7], cshift_b], [lgT_b])

    def resid_update(t, dch, p_, pb_, g_off, stats=None):
        t0 = t * TT
        x_, xb = nxc()
        load(x_[:], xsrc[dch][t][dch, :, t0:t0 + TT], xb, xs_bt[dch][t])
        dve(lambda e: e.scalar_tensor_tensor(out=x_[:], in0=p_[:, 0:TT], scalar=mod[:, g_off + dch:g_off + dch + 1], in1=x_[:], op0=ALU.mult, op1=ALU.add),
            [pb_, xb, mod_b], [xb])
        xstore(xs[dch, :, t0:t0 + TT], x_[:], xb, xs_bt[dch][t])
        xsrc[dch][t] = xs
        if stats is None:
            return None
        sp_, spb_ = stats
        i = dch % 2
        act(lambda e: e.activation(out=sqb[i][:], in_=x_[:], func=AF.Square), [xb], [sq_b[i]])

        def deferred():
            pe(lambda e: e.matmul(sp_[:, 0:TT], lhsT=ones[:], rhs=sqb[i][:], start=(dch == 0), stop=(dch == KD - 1)), [sq_b[i], con_b], [spb_])
        return deferred

    def gemm_fm(name, idx, kc, src, srcb, j, p_, pb_):
        pass

    for l in range(L):
        moe = (l % 2 == 1)
        load(mod[:], ada_tab[:, l, :], mod_b)
        dve(lambda e, l=l: e.tensor_tensor(out=mod[:], in0=modb[:], in1=mod[:], op=ALU.add), [modb_b, mod_b], [mod_b])
        for j in (1, 4):
            dve(lambda e, j=j: e.tensor_scalar(out=mod[:, j * KD:(j + 1) * KD], in0=mod[:, j * KD:(j + 1) * KD], scalar1=1.0, scalar2=None, op0=ALU.add), [mod_b], [mod_b])
        if l > 0:
            load(gwa[:], gaw[:, l], gw_b, eng="pool")
            load(gwx[:], gxw[:, l], gw_b, eng="pool")
        if moe:
            load(rw[:], router[:, l // 2], rw_b)
            pcs, pcsb = nps(6, 8)
            for k in range(KD):
                pe(lambda e, pcs=pcs, k=k: e.matmul(pcs[0:E, 0:1], lhsT=rw[:, k, :], rhs=mod[:, 3 * KD + k:3 * KD + k + 1], start=(k == 0), stop=(k == KD - 1)), [rw_b, mod_b], [pcsb])
            dve(lambda e, pcs=pcs: e.tensor_copy(out=cshift[:], in_=pcs[0:E, 0:1]), [pcsb], [cshift_b])
        dve(lambda e: e.memset(Sst[:], 0.0), [], S_b)
        dve(lambda e: e.memset(Sbf[:], 0.0), [], S_b)
        dve(lambda e: e.memset(hst[:], 0.0), [], h_b)
        dve(lambda e: e.memset(halo[:], 0.0), [], halo_b)

        for t in range(NT):
            t0 = t * TT
            rope_tile(t)
            norm_to_hn(t, 1 * KD, 0 * KD)
            for h in range(H):
                w_, wb_ = wload(f"win{l}", 3 * h, KD)
                for m in range(MS):
                    p_, pb_ = nps()
                    for k in range(KD):
                        pe(lambda e, p_=p_, w_=w_, m=m, k=k: e.matmul(p_[:, 0:GW], lhsT=hnT[:, k, m * 128:(m + 1) * 128], rhs=w_[:, k, :], start=(k == 0), stop=(k == KD - 1)),
                           [hn_b, wb_], [pb_])
                    ci = m
                    pv = p_[:, 0:GW].rearrange("p (a b c) -> p a b c", a=2, b=2)
                    t1, tb1 = ntmp(); t2, tb2 = ntmp()
                    cosb = cosT[:, ci, :].unsqueeze(1).broadcast_to([128, 2, 64]); sinb = sinT[:, ci, :].unsqueeze(1).broadcast_to([128, 2, 64])
                    a1 = t1[:, 0:128].rearrange("p (a c) -> p a c", a=2); a2 = t1[:, 128:256].rearrange("p (a c) -> p a c", a=2)
                    b1 = t2[:, 0:128].rearrange("p (a c) -> p a c", a=2); b2 = t2[:, 128:256].rearrange("p (a c) -> p a c", a=2)
                    qv = qks[:].rearrange("p a (b c) -> p a b c", b=2)
                    dve(lambda e, pv=pv, a1=a1, cosb=cosb: e.tensor_tensor(out=a1, in0=pv[:, :, 0, :], in1=cosb, op=ALU.mult), [pb_, rope_b], [tb1])
                    dve(lambda e, pv=pv, a2=a2, sinb=sinb: e.tensor_tensor(out=a2, in0=pv[:, :, 1, :], in1=sinb, op=ALU.mult), [pb_, rope_b], [tb1])
                    dve(lambda e, pv=pv, b1=b1, sinb=sinb: e.tensor_tensor(out=b1, in0=pv[:, :, 0, :], in1=sinb, op=ALU.mult), [pb_, rope_b], [tb2])
                    dve(lambda e, pv=pv, b2=b2, cosb=cosb: e.tensor_tensor(out=b2, in0=pv[:, :, 1, :], in1=cosb, op=ALU.mult), [pb_, rope_b], [tb2])
                    dve(lambda e, qv=qv, a1=a1, a2=a2: e.tensor_tensor(out=qv[:, :, 0, :], in0=a1, in1=a2, op=ALU.subtract), [tb1], [qks_b])
                    dve(lambda e, qv=qv, b1=b1, b2=b2: e.tensor_tensor(out=qv[:, :, 1, :], in0=b1, in1=b2, op=ALU.add), [tb2], [qks_b])
                    dve(lambda e, m=m, h=h: e.tensor_scalar(out=kds[:, m, :], in0=qks[:, 1, :], scalar1=kdec[:, h:h + 1], scalar2=None, op0=ALU.mult), [qks_b, con_b], [kds_b])
                    pt, ptb = nps(6, 8)
                    ptv = pt[:].bitcast(BF16)
                    pe(lambda e, ptv=ptv: e.transpose(out=ptv[:, 0:128], in_=qks[:, 0, :], identity=ident[:]), [qks_b, con_b], [ptb])
                    pe(lambda e, ptv=ptv: e.transpose(out=ptv[:, 128:256], in_=qks[:, 1, :], identity=ident[:]), [qks_b, con_b], [ptb])
                    act(lambda e, ptv=ptv, m=m: e.activation(out=qTs[:, 0, m * 128:(m + 1) * 128], in_=ptv[:, 0:128], func=AF.Copy), [ptb], [qT_b])
                    dve(lambda e, ptv=ptv, m=m, h=h: e.tensor_tensor(out=qTs[:, 1, m * 128:(m + 1) * 128], in0=ptv[:, 0:128], in1=qdec[:, h, :], op=ALU.mult), [ptb, con_b], [qT_b])
                    act(lambda e, ptv=ptv, m=m: e.activation(out=qTs[:, 2, m * 128:(m + 1) * 128], in_=ptv[:, 128:256], func=AF.Copy), [ptb], [qT_b])
                w_, wb_ = wload(f"win{l}", 3 * h + 1, KD)
                for m in range(MS):
                    p_, pb_ = nps()
                    for k in range(KD):
                        pe(lambda e, p_=p_, w_=w_, m=m, k=k: e.matmul(p_[:, 0:GW], lhsT=hnT[:, k, m * 128:(m + 1) * 128], rhs=w_[:, k, :], start=(k == 0), stop=(k == KD - 1)),
                           [hn_b, wb_], [pb_])
                    act(lambda e, p_=p_, m=m: e.activation(out=vs[:, m, :], in_=p_[:, 0:GW], func=AF.Copy), [pb_], [vs_b])
                w_, wb_ = wload(f"win{l}", 3 * h + 2, KD)
                for m in range(MS):
                    p_, pb_ = nps()
                    for k in range(KD):
                        pe(lambda e, p_=p_, w_=w_, m=m, k=k: e.matmul(p_[:, 0:GW], lhsT=hnT[:, k, m * 128:(m + 1) * 128], rhs=w_[:, k, :], start=(k == 0), stop=(k == KD - 1)),
                           [hn_b, wb_], [pb_])
                    act(lambda e, p_=p_, m=m: e.activation(out=gs[:, m, :], in_=p_[:, 0:GW], func=AF.Silu), [pb_], [gs_b])
                for m in range(MS):
                    msl = slice(m * 128, (m + 1) * 128)
                    i2 = m % 2
                    pS, pSb = nps()
                    pe(lambda e, pS=pS, msl=msl: e.matmul(pS[:, 0:128], lhsT=qTs[:, 2, msl], rhs=qTs[:, 0, msl], start=True, stop=True), [qT_b], [pSb])
                    dve(lambda e, pS=pS, i2=i2, h=h: e.tensor_tensor(out=sTs[i2][:], in0=pS[:, 0:128], in1=maskT[:, h, :], op=ALU.mult), [pSb, con_b], [sT_b[i2]])
                    pO, pOb = nps()
                    pe(lambda e, pO=pO, i2=i2, m=m: e.matmul(pO[:, 0:256], lhsT=sTs[i2][:], rhs=vs[:, m, :], start=True, stop=False), [sT_b[i2], vs_b], [pOb])
                    pe(lambda e, pO=pO, msl=msl, h=h: e.matmul(pO[:, 0:256], lhsT=qTs[:, 1, msl], rhs=Sbf[:, h, :], start=False, stop=True), [qT_b, S_b[h]], [pOb])
                    pD, pDb = nps()
                    pe(lambda e, pD=pD, m=m: e.matmul(pD[:, 0:256], lhsT=kds[:, m, :], rhs=vs[:, m, :], start=True, stop=True), [kds_b, vs_b], [pDb])
                    dve(lambda e, pD=pD, h=h: e.scalar_tensor_tensor(out=Sst[:, h, :], in0=Sst[:, h, :], scalar=gC[:, h:h + 1], in1=pD[:, 0:256], op0=ALU.mult, op1=ALU.add),
                        [pDb, con_b, S_b[h]], [S_b[h]])
                    act(lambda e, h=h: e.activation(out=Sbf[:, h, :], in_=Sst[:, h, :], func=AF.Copy), [S_b[h]], [S_b[h]])
                    yr, yrb_ = yrs[i2], yr_b[i2]
                    dve(lambda e, pO=pO: e.bn_stats(out=bst[:, 0:6], in_=pO[:, 0:256]), [pOb], [bst_b])
                    dve(lambda e: e.bn_aggr(out=small[:, 0:2], in_=bst[:, 0:6]), [bst_b], [small_b])
                    act(lambda e: e.activation(out=small[:, 2:3], in_=small[:, 1:2], func=AF.Sqrt, bias=float(EPS), scale=1.0), [small_b], [small_b])
                    dve(lambda e: e.reciprocal(out=small[:, 2:3], in_=small[:, 2:3]), [small_b], [small_b])
                    dve(lambda e, pO=pO, yr=yr: e.tensor_scalar(out=yr[:], in0=pO[:, 0:256], scalar1=small[:, 0:1], scalar2=small[:, 2:3], op0=ALU.subtract, op1=ALU.mult),
                        [pOb, small_b], [yrb_])
                    dve(lambda e, yr=yr, i2=i2, m=m: e.tensor_tensor(out=yrb[i2][:], in0=yr[:], in1=gs[:, m, :], op=ALU.mult), [yrb_, gs_b], [yrb_b[i2]])
                    pt, ptb = nps(6, 8)
                    ptv = pt[:].bitcast(BF16)
                    for jj in range(2):
                        pe(lambda e, ptv=ptv, i2=i2, jj=jj: e.transpose(out=ptv[:, jj * 128:(jj + 1) * 128], in_=yrb[i2][:, jj * 128:(jj + 1) * 128], identity=ident[:]),
                           [yrb_b[i2], con_b], [ptb])
                    for jj in range(2):
                        act(lambda e, ptv=ptv, jj=jj, h=h, msl=msl: e.activation(out=yT[:, 2 * h + jj, msl], in_=ptv[:, jj * 128:(jj + 1) * 128], func=AF.Identity, scale=rngs[:, l, 2 * h + jj:2 * h + jj + 1]), [ptb, con_b], [y_b])
            ssq, ssqb = pst[7], ps_b[7]

            def lru_A(n, l=l):
                w_, wb_ = wload(f"win{l}", 3 * H + n, KD)
                pG, pGb = nps(); pX, pXb = nps()
                for k in range(KD):
                    pe(lambda e, pG=pG, w_=w_, k=k: e.matmul(pG[:, 0:TT], lhsT=w_[:, k, 0:128], rhs=hnT[:, k, :], start=(k == 0), stop=(k == KD - 1)), [hn_b, wb_], [pGb])
                for k in range(KD):
                    pe(lambda e, pX=pX, w_=w_, k=k: e.matmul(pX[:, 0:TT], lhsT=w_[:, k, 128:256], rhs=hnT[:, k, :], start=(k == 0), stop=(k == KD - 1)), [hn_b, wb_], [pXb])
                tg, tgb = ltmp[2 * (n % 2)], ltmp_b[2 * (n % 2)]
                xcv, xcvb = ltmp[2 * (n % 2) + 1], ltmp_b[2 * (n % 2) + 1]
                act(lambda e: e.activation(out=tg[:], in_=pG[:, 0:TT], func=AF.Square), [pGb], [tgb])
                dve(lambda e: e.tensor_scalar(out=tg[:], in0=tg[:], scalar1=0.044715, scalar2=1.0, op0=ALU.mult, op1=ALU.add), [tgb], [tgb])
                dve(lambda e: e.tensor_tensor(out=tg[:], in0=tg[:], in1=pG[:, 0:TT], op=ALU.mult), [tgb, pGb], [tgb])
                act(lambda e: e.activation(out=tg[:], in_=tg[:], func=AF.Sigmoid, scale=float(2.0 * np.sqrt(2.0 / np.pi))), [tgb], [tgb])
                dve(lambda e: e.tensor_tensor(out=tg[:], in0=tg[:], in1=pG[:, 0:TT], op=ALU.mult), [tgb, pGb], [tgb])
                lx_, lxb_ = lxt[n % 2], lxt_b[n % 2]
                dve(lambda e: e.tensor_copy(out=lx_[:, 0:3], in_=halo[:, n, :]), [halo_b[n]], [lxb_])
                act(lambda e: e.activation(out=lx_[:, 3:3 + TT], in_=pX[:, 0:TT], func=AF.Copy), [pXb], [lxb_])
                act(lambda e: e.activation(out=xcv[:], in_=lx_[:, 3:3 + TT], func=AF.Identity, bias=lpar["convb"][:, l, n:n + 1], scale=cw[:, l, 3, n:n + 1]),
                    [lxb_, lpar_b, con_b], [xcvb])
                for j in range(3):
                    dve(lambda e, j=j: e.scalar_tensor_tensor(out=xcv[:], in0=lx_[:, j:j + TT], scalar=cw[:, l, j, n:n + 1], in1=xcv[:], op0=ALU.mult, op1=ALU.add),
                        [lxb_, con_b, xcvb], [xcvb])
                dve(lambda e: e.tensor_copy(out=halo[:, n, :], in_=lx_[:, TT:TT + 3]), [lxb_], [halo_b[n]])
                act(lambda e: e.activation(out=xcb[n % 2][:], in_=xcv[:], func=AF.Copy), [xcvb], [xcb_b[n % 2]])

            def lru_B(n, l=l):
                i2 = n % 2
                tg, tgb = ltmp[2 * i2], ltmp_b[2 * i2]
                xcv, xcvb = ltmp[2 * i2 + 1], ltmp_b[2 * i2 + 1]
                pR, pRb = nps(); pI, pIb = nps()
                pe(lambda e: e.matmul(pR[:, 0:TT], lhsT=gwa[:, n, :], rhs=xcb[i2][:], start=True, stop=True), [gw_b, xcb_b[i2]], [pRb])
                pe(lambda e: e.matmul(pI[:, 0:TT], lhsT=gwx[:, n, :], rhs=xcb[i2][:], start=True, stop=True), [gw_b, xcb_b[i2]], [pIb])
                tu, tub = ntmp(); ta, tab = ntmp(); ti, tib = ntmp()
                act(lambda e: e.activation(out=ta[:], in_=pR[:, 0:TT], func=AF.Sigmoid, bias=lpar["gab"][:, l, n:n + 1], scale=1.0), [pRb, lpar_b], [tab])
                act(lambda e: e.activation(out=ti[:], in_=pI[:, 0:TT], func=AF.Sigmoid, bias=lpar["gxb"][:, l, n:n + 1], scale=1.0), [pIb, lpar_b], [tib])
                dve(lambda e: e.tensor_tensor(out=ti[:], in0=ti[:], in1=xcv[:], op=ALU.mult), [tib, xcvb], [tib])
                act(lambda e: e.activation(out=tu[:], in_=ta[:], func=AF.Exp, scale=cneg2[:, l, n:n + 1]), [tab, lpar_b], [tub])
                act(lambda e: e.activation(out=ta[:], in_=ta[:], func=AF.Exp, scale=cneg[:, l, n:n + 1]), [tab, lpar_b], [tab])
                dve(lambda e: e.tensor_scalar(out=tu[:], in0=tu[:], scalar1=-1.0, scalar2=1.0, op0=ALU.mult, op1=ALU.add), [tub], [tub])
                dve(lambda e: e.tensor_scalar(out=tu[:], in0=tu[:], scalar1=0.0, scalar2=None, op0=ALU.max), [tub], [tub])
                act(lambda e: e.activation(out=tu[:], in_=tu[:], func=AF.Sqrt), [tub], [tub])
                dve(lambda e: e.tensor_tensor(out=tu[:], in0=tu[:], in1=ti[:], op=ALU.mult), [tub, tib], [tub])
                dve(lambda e: e.tensor_tensor_scan(out=ti[:], data0=ta[:], data1=tu[:], initial=hst[:, n:n + 1], op0=ALU.mult, op1=ALU.add),
                    [tab, tub, h_b[n]], [tib])
                dve(lambda e: e.tensor_copy(out=hst[:, n:n + 1], in_=ti[:, TT - 1:TT]), [tib], [h_b[n]])
                act(lambda e: e.activation(out=sqb[i2][:], in_=ti[:], func=AF.Square), [tib], [sq_b[i2]])
                dve(lambda e: e.scalar_tensor_tensor(out=yT[:, 2 * H + n, :], in0=ti[:], scalar=lpar["lng"][:, l, n:n + 1], in1=tg[:], op0=ALU.mult, op1=ALU.mult),
                    [tib, tgb, lpar_b], [y_b])

            def lru_C(n):
                i2 = n % 2
                pe(lambda e: e.matmul(ssq[:, 0:TT], lhsT=ones[:], rhs=sqb[i2][:], start=(n == 0), stop=(n == NBL - 1)), [sq_b[i2], con_b], [ssqb])

            for n in range(NBL + 2):
                if n < NBL:
                    lru_A(n)
                if 0 <= n - 1 < NBL:
                    lru_B(n - 1)
                if 0 <= n - 2 < NBL:
                    lru_C(n - 2)
            act(lambda e: e.activation(out=rstd[:], in_=ssq[:, 0:TT], func=AF.Sqrt, bias=float(EPS), scale=float(1.0 / LW)), [ssqb], [rstd_b])
            dve(lambda e: e.reciprocal(out=rstd[:], in_=rstd[:]), [rstd_b], [rstd_b])
            for n in range(NBL):
                dve(lambda e, n=n: e.tensor_tensor(out=yT[:, 2 * H + n, :], in0=yT[:, 2 * H + n, :], in1=rstd[:], op=ALU.mult), [rstd_b, y_b], [y_b])
            stat = (pst[6], ps_b[6])
            pend = None
            for g in range(DG):
                w_, wb_ = wload(f"wout{l}", g, MKC)
                for j in range(2):
                    p_, pb_ = nps()
                    for k in range(MKC):
                        pe(lambda e, p_=p_, w_=w_, j=j, k=k: e.matmul(p_[:, 0:TT], lhsT=w_[:, k, j * 128:(j + 1) * 128], rhs=yT[:, k, :], start=(k == 0), stop=(k == MKC - 1)),
                           [y_b, wb_], [pb_])
                    if pend is not None:
                        pend()
                    pend = resid_update(t, 2 * g + j, p_, pb_, 2 * KD, stats=stat)
            if pend is not None:
                pend()
            norm_to_hn(t, 4 * KD, 3 * KD, stats=stat, router=moe)
            if not moe:
                for half in range(2):
                    for gg in range(FH // 2):
                        g = half * (FH // 2) + gg
                        wg_, wgb = wload(f"wg{l}", g, KD)
                        wu_, wub = wload(f"wu{l}", g, KD)
                        for j in range(2):
                            pg, pgb = nps(); pu, pub = nps()
                            for k in range(KD):
                                pe(lambda e, pg=pg, wg_=wg_, j=j, k=k: e.matmul(pg[:, 0:TT], lhsT=wg_[:, k, j * 128:(j + 1) * 128], rhs=hnT[:, k, :], start=(k == 0), stop=(k == KD - 1)), [hn_b, wgb], [pgb])
                            for k in range(KD):
                                pe(lambda e, pu=pu, wu_=wu_, j=j, k=k: e.matmul(pu[:, 0:TT], lhsT=wu_[:, k, j * 128:(j + 1) * 128], rhs=hnT[:, k, :], start=(k == 0), stop=(k == KD - 1)), [hn_b, wub], [pub])
                            t_, tb = ntmp()
                            act(lambda e, pg=pg, t_=t_: e.activation(out=t_[:], in_=pg[:, 0:TT], func=AF.Silu), [pgb], [tb])
                            dve(lambda e, pu=pu, t_=t_, c=2 * gg + j: e.tensor_tensor(out=yT[:, c, :], in0=t_[:], in1=pu[:, 0:TT], op=ALU.mult), [pub, tb], [y_b])
                    for g in range(DG):
                        w_, wb_ = wload(f"wd{l}", half * DG + g, FH)
                        for j in range(2):
                            p_, pb_ = nps()
                            for k in range(FH):
                                pe(lambda e, p_=p_, w_=w_, j=j, k=k: e.matmul(p_[:, 0:TT], lhsT=w_[:, k, j * 128:(j + 1) * 128], rhs=yT[:, k, :], start=(k == 0), stop=(k == FH - 1)), [y_b, wb_], [pb_])
                            resid_update(t, 2 * g + j, p_, pb_, 5 * KD)
            else:
                for m in range(MS):
                    pl, plb = nps(0, 6)
                    pe(lambda e, pl=pl, m=m: e.transpose(out=pl[:, 0:E], in_=lgT[:, m * 128:(m + 1) * 128], identity=identf[0:E, 0:E]), [lgT_b, con_b], [plb])
                    lg = small[:, 16:16 + E]; m1 = small[:, 32:33]; m2 = small[:, 33:34]; eq1 = small[:, 40:40 + E]; w1 = small[:, 34:35]; w2 = small[:, 35:36]
                    dve(lambda e, pl=pl: e.tensor_copy(out=small[:, 16:16 + E], in_=pl[:, 0:E]), [plb], [small_b])
                    dve(lambda e: e.tensor_reduce(out=small[:, 32:33], in_=small[:, 16:16 + E], axis=mybir.AxisListType.X, op=ALU.max), [small_b], [small_b])
                    dve(lambda e: e.tensor_scalar(out=small[:, 40:40 + E], in0=small[:, 16:16 + E], scalar1=small[:, 32:33], scalar2=None, op0=ALU.is_ge), [small_b], [small_b])
                    dve(lambda e: e.scalar_tensor_tensor(out=small[:, 48:48 + E], in0=small[:, 40:40 + E], scalar=-1e30, in1=small[:, 16:16 + E], op0=ALU.mult, op1=ALU.add), [small_b], [small_b])
                    dve(lambda e: e.tensor_reduce(out=small[:, 33:34], in_=small[:, 48:48 + E], axis=mybir.AxisListType.X, op=ALU.max), [small_b], [small_b])
                    dve(lambda e: e.tensor_scalar(out=small[:, 56:56 + E], in0=small[:, 48:48 + E], scalar1=small[:, 33:34], scalar2=None, op0=ALU.is_ge), [small_b], [small_b])
                    dve(lambda e: e.tensor_tensor(out=small[:, 34:35], in0=small[:, 32:33], in1=small[:, 33:34], op=ALU.subtract), [small_b], [small_b])
                    act(lambda e: e.activation(out=small[:, 34:35], in_=small[:, 34:35], func=AF.Sigmoid), [small_b], [small_b])
                    dve(lambda e: e.tensor_scalar(out=small[:, 35:36], in0=small[:, 34:35], scalar1=-1.0, scalar2=1.0, op0=ALU.mult, op1=ALU.add), [small_b], [small_b])
                    dve(lambda e, m=m: e.tensor_scalar(out=comb[:, m, :], in0=small[:, 40:40 + E], scalar1=small[:, 34:35], scalar2=None, op0=ALU.mult), [small_b], [comb_b])
                    dve(lambda e, m=m: e.scalar_tensor_tensor(out=comb[:, m, :], in0=small[:, 56:56 + E], scalar=small[:, 35:36], in1=comb[:, m, :], op0=ALU.mult, op1=ALU.add), [small_b, comb_b], [comb_b])
                    pc, pcb = nps(6, 8)
                    pe(lambda e, pc=pc, m=m: e.transpose(out=pc[0:E, 0:128], in_=comb[:, m, :], identity=identf[:]), [comb_b, con_b], [pcb])
                    dve(lambda e, pc=pc, m=m: e.tensor_copy(out=combT[:, m * 128:(m + 1) * 128], in_=pc[0:E, 0:128]), [pcb], [combT_b])
                NG2 = DE // GW
                for qtr in range(NQ):
                    for el in range(EQ):
                        ex = qtr * EQ + el
                        pb2, pb2b = nps(6, 8)
                        pe(lambda e, pb2=pb2, ex=ex: e.matmul(pb2[:, 0:TT], lhsT=sel[:, ex, :], rhs=combT[:, :], start=True, stop=True), [sel_b, combT_b], [pb2b])
                        dve(lambda e, pb2=pb2: e.tensor_copy(out=combB[:], in_=pb2[:, 0:TT]), [pb2b], [combB_b])
                        for gg in range(NG2):
                            wg_, wgb = wload(f"wg{l}", ex * NG2 + gg, KD)
                            wu_, wub = wload(f"wu{l}", ex * NG2 + gg, KD)
                            for j in range(2):
                                pg, pgb = nps(); pu, pub = nps()
                                for k in range(KD):
                                    pe(lambda e, pg=pg, wg_=wg_, j=j, k=k: e.matmul(pg[:, 0:TT], lhsT=wg_[:, k, j * 128:(j + 1) * 128], rhs=hnT[:, k, :], start=(k == 0), stop=(k == KD - 1)), [hn_b, wgb], [pgb])
                                for k in range(KD):
                                    pe(lambda e, pu=pu, wu_=wu_, j=j, k=k: e.matmul(pu[:, 0:TT], lhsT=wu_[:, k, j * 128:(j + 1) * 128], rhs=hnT[:, k, :], start=(k == 0), stop=(k == KD - 1)), [hn_b, wub], [pub])
                                t_, tb = ntmp()
                                act(lambda e, pg=pg, t_=t_: e.activation(out=t_[:], in_=pg[:, 0:TT], func=AF.Silu), [pgb], [tb])
                                dve(lambda e, t_=t_, ex=ex: e.tensor_tensor(out=t_[:], in0=t_[:], in1=combB[:], op=ALU.mult), [tb, combB_b], [tb])
                                dve(lambda e, pu=pu, t_=t_, c=el * DEC + 2 * gg + j: e.tensor_tensor(out=yT[:, c, :], in0=t_[:], in1=pu[:, 0:TT], op=ALU.mult), [pub, tb], [y_b])
                    for g in range(DG):
                        w_, wb_ = wload(f"wd{l}", qtr * DG + g, QC)
                        for j in range(2):
                            p_, pb_ = nps()
                            for k in range(QC):
                                pe(lambda e, p_=p_, w_=w_, j=j, k=k: e.matmul(p_[:, 0:TT], lhsT=w_[:, k, j * 128:(j + 1) * 128], rhs=yT[:, k, :], start=(k == 0), stop=(k == QC - 1)), [y_b, wb_], [pb_])
                            resid_update(t, 2 * g + j, p_, pb_, 5 * KD)

    out_b = Buf("out")
    for t in range(NT):
        t0 = t * TT
        pst_, pstb = nps(6, 8)
        for k in range(KD):
            x_, xb = nxc()
            load(x_[:], xsrc[k][t][k, :, t0:t0 + TT], xb, xs_bt[k][t])
            i = k % 2
            act(lambda e, x_=x_, i=i: e.activation(out=sqb[i][:], in_=x_[:], func=AF.Square), [xb], [sq_b[i]])
            pe(lambda e, i=i, k=k, pst_=pst_: e.matmul(pst_[:, 0:TT], lhsT=ones[:], rhs=sqb[i][:], start=(k == 0), stop=(k == KD - 1)), [sq_b[i], con_b], [pstb])
        act(lambda e, pst_=pst_: e.activation(out=rstd[:], in_=pst_[:, 0:TT], func=AF.Sqrt, bias=float(EPS), scale=float(1.0 / D)), [pstb], [rstd_b])
        dve(lambda e: e.reciprocal(out=rstd[:], in_=rstd[:]), [rstd_b], [rstd_b])
        for k in range(KD):
            x_, xb = nxc()
            load(x_[:], xsrc[k][t][k, :, t0:t0 + TT], xb, xs_bt[k][t])
            dve(lambda e, x_=x_, k=k: e.scalar_tensor_tensor(out=x_[:], in0=x_[:], scalar=fngs[:, k:k + 1], in1=rstd[:], op0=ALU.mult, op1=ALU.mult), [xb, rstd_b, con_b], [xb])
            xstore(outT[k, :, t0:t0 + TT], x_[:], xb, out_b)

    S.emit(nc, st)
    st.close()
    return nc


def _tile_w(w, gw=GW):
    K, N = w.shape
    return np.ascontiguousarray(w.reshape(K // 128, 128, N // gw, gw).transpose(2, 1, 0, 3))


def _pp(v, nb):
    v = np.asarray(v)
    lead = v.shape[:-1]
    return np.ascontiguousarray(np.moveaxis(v.reshape(*lead, nb, 128), -1, 0))


def make_inputs(cfg, inp):
    D, H, LW, FF, E, DE, L, SEQ, TT, B = (cfg[k] for k in ("D", "H", "LW", "FF", "E", "DE", "L", "SEQ", "TT", "B"))
    KD = D // 128; Vw = H * 256; NBL = LW // 128; QK = H * 128; NCH = SEQ // 128
    f32 = np.float32
    shared = {}
    aw = np.asarray(inp["ada_w"], f32)
    shared["ada_w"] = np.ascontiguousarray(aw.reshape(KD, 128, 6 * KD, 128).transpose(2, 1, 0, 3))
    shared["ada_b"] = _pp(np.asarray(inp["ada_b"], f32), 6 * KD)
    shared["ada_tab"] = _pp(np.asarray(inp["ada_table"], f32).reshape(L, 6 * D), 6 * KD)
    shared["convw"] = _pp(np.asarray(inp["conv_w"], f32), NBL)
    for n, k in (("convb", "conv_b"), ("gab", "gate_a_b"), ("gxb", "gate_x_b"), ("lam", "lru_lambda"), ("lng", "lru_norm_g")):
        shared[n] = _pp(np.asarray(inp[k], f32), NBL)
    shared["rng"] = _pp(np.asarray(inp["ret_norm_g"], f32), 2 * H)
    shared["fng"] = _pp(np.asarray(inp["final_norm_g"], f32), KD)
    shared["gaw"] = np.ascontiguousarray(np.asarray(inp["gate_a_w"], f32).transpose(2, 0, 1, 3))
    shared["gxw"] = np.ascontiguousarray(np.asarray(inp["gate_x_w"], f32).transpose(2, 0, 1, 3))
    rw = np.asarray(inp["router_w"], f32)
    shared["router"] = np.ascontiguousarray(rw.reshape(rw.shape[0], KD, 128, E).transpose(2, 0, 1, 3))
    lg = np.log1p(-np.exp2(-5.0 - np.arange(H, dtype=np.float64)))
    idx = np.arange(128, dtype=np.float64)
    rel = idx[None, :] - idx[:, None]
    maskT = np.where(rel[None] >= 0, np.exp(lg[:, None, None] * np.maximum(rel[None], 0)), 0.0) * (128 ** -0.5)
    shared["maskT"] = np.ascontiguousarray(maskT.transpose(1, 0, 2)).astype(f32)
    shared["qdec"] = np.ascontiguousarray(np.broadcast_to(np.exp(lg[:, None] * (idx[None, :] + 1.0))[None], (128, H, 128))).astype(f32)
    shared["kdec"] = np.ascontiguousarray((np.exp(lg[None, :] * (127.0 - idx[:, None])) * (128 ** -0.5))).astype(f32)
    shared["gC"] = np.ascontiguousarray(np.broadcast_to(np.exp(lg * 128.0)[None], (128, H))).astype(f32)
    invf = np.exp2(-np.arange(0, 128, 2, dtype=f32) / f32(128) * np.log2(f32(10000.0))).astype(f32)
    shared["invf"] = np.ascontiguousarray(np.broadcast_to(invf[None], (128, 64)))
    shared["ident"] = np.eye(128, dtype=f32)
    S0, S1, S2, S3, S4 = QK, 2 * QK, 2 * QK + Vw, 2 * QK + 2 * Vw, 2 * QK + 2 * Vw + LW
    for l in range(L):
        w = np.asarray(inp["w_in"][l], f32)
        cols = []
        for h in range(H):
            cols += [w[:, h * 128:(h + 1) * 128], w[:, S0 + h * 128:S0 + (h + 1) * 128], w[:, S1 + h * 256:S1 + (h + 1) * 256], w[:, S2 + h * 256:S2 + (h + 1) * 256]]
        for n in range(NBL):
            cols += [w[:, S3 + n * 128:S3 + (n + 1) * 128], w[:, S4 + n * 128:S4 + (n + 1) * 128]]
        shared[f"win{l}"] = _tile_w(np.concatenate(cols, axis=1))
        shared[f"wout{l}"] = _tile_w(np.asarray(inp["w_out"][l], f32))
        if l % 2 == 0:
            i = l // 2
            shared[f"wg{l}"] = _tile_w(np.asarray(inp["ffn_w_gate"][i], f32)); shared[f"wu{l}"] = _tile_w(np.asarray(inp["ffn_w_up"][i], f32))
            wd = np.asarray(inp["ffn_w_down"][i], f32)
            shared[f"wd{l}"] = np.concatenate([_tile_w(wd[hh * FF // 2:(hh + 1) * FF // 2]) for hh in range(2)], axis=0)
        else:
            i = l // 2
            mg = np.asarray(inp["moe_w_gate"][i], f32); mu = np.asarray(inp["moe_w_up"][i], f32); md = np.asarray(inp["moe_w_down"][i], f32)
            shared[f"wg{l}"] = np.concatenate([_tile_w(mg[e]) for e in range(E)], axis=0)
            shared[f"wu{l}"] = np.concatenate([_tile_w(mu[e]) for e in range(E)], axis=0)
            mdf = md.reshape(E * DE, D)
            q = E * DE // 4
            shared[f"wd{l}"] = np.concatenate([_tile_w(mdf[qq * q:(qq + 1) * q]) for qq in range(4)], axis=0)
    maps = []
    x = np.asarray(inp["x"], f32); c = np.asarray(inp["c"], f32); pos = np.asarray(inp["positions"], np.int32)
    for b in range(B):
        m = dict(shared)
        m["xT"] = np.ascontiguousarray(x[b].T.reshape(KD, 128, SEQ))
        m["cT"] = _pp(c[b], KD)
        m["pos"] = np.ascontiguousarray(pos[b].reshape(NCH, 128).T)
        maps.append(m)
    return maps


def run(cfg, inp):
    nc = build(cfg)
    maps = make_inputs(cfg, inp)
    B = cfg["B"]
    res = run_bass_kernel_spmd(nc, maps, core_ids=list(range(B)))
    D, SEQ = cfg["D"], cfg["SEQ"]
    out = np.stack([np.asarray(res.results[b]["outT"]).reshape(D, SEQ).T for b in range(B)], axis=0)
    return np.ascontiguousarray(out.astype(np.float32))


def kernel(**inputs):
    return run(FULL, inputs)
```
